# Optimizing a Trainium2 kernel written in Bass

```python
import jax, jax.numpy as jnp
from jax import lax
import numpy as np

D_MODEL = 1024
BATCH = 8
SEQ = 2048
DEPTH = 1

HEAD_DIM = 64
N_HEADS_A = D_MODEL // (2 * HEAD_DIM)
N_HEADS_B = D_MODEL // (2 * HEAD_DIM)
WIDTH_A = N_HEADS_A * HEAD_DIM
WIDTH_B = N_HEADS_B * HEAD_DIM
MIX_WIDTH = WIDTH_A + WIDTH_B
IN_WIDTH = 2 * WIDTH_A + 3 * WIDTH_B
CHUNK = 128
MOBA_BLOCK = 256
MOBA_TOPK = 3
Q_BLOCK = 128
D_FF = -(-8 * D_MODEL // (3 * 256)) * 256
EPS = 1e-6

kernel_name = "hymba_sgu_moba_hybrid_layer"


def rms_norm(x, g):
    xf = x.astype(jnp.float32)
    y = xf * lax.rsqrt(jnp.mean(xf * xf, axis=-1, keepdims=True) + EPS)
    return (y * g.astype(jnp.float32)).astype(x.dtype)


def layer_norm(x, g, b):
    xf = x.astype(jnp.float32)
    mu = jnp.mean(xf, axis=-1, keepdims=True)
    var = jnp.mean(jnp.square(xf - mu), axis=-1, keepdims=True)
    y = (xf - mu) * lax.rsqrt(var + EPS)
    return (y * g.astype(jnp.float32) + b.astype(jnp.float32)).astype(x.dtype)


def chunked_sgu(u, v, ln_g, ln_b, w_s, b_s):
    B, S, _ = u.shape
    u = jax.nn.gelu(u)
    v = jax.nn.gelu(v).reshape(B, S, N_HEADS_A, HEAD_DIM)
    v = layer_norm(v, ln_g.reshape(N_HEADS_A, HEAD_DIM), ln_b.reshape(N_HEADS_A, HEAD_DIM))
    v = v.reshape(B, S // CHUNK, CHUNK, N_HEADS_A, HEAD_DIM)
    causal = jnp.tril(jnp.ones((CHUNK, CHUNK), dtype=bool))
    w = jnp.where(causal[None], w_s, 0)
    mixed = jnp.einsum('hts,bcshd->bcthd', w, v) + b_s.T[None, None, :, :, None]
    return u * mixed.reshape(B, S, WIDTH_A)


def to_qblocks(a):
    B, H, S = a.shape[:3]
    a = a.reshape(B, H, S // Q_BLOCK, Q_BLOCK, *a.shape[3:])
    return jnp.moveaxis(a, 2, 0)


def moba_attention(q, k, v, q_g, k_g):
    B, S, _ = q.shape
    H = N_HEADS_B
    q = rms_norm(q.reshape(B, S, H, HEAD_DIM), q_g).transpose(0, 2, 1, 3)
    k = rms_norm(k.reshape(B, S, H, HEAD_DIM), k_g).transpose(0, 2, 1, 3)
    v = v.reshape(B, S, H, HEAD_DIM).transpose(0, 2, 1, 3)
    nb = -(-S // MOBA_BLOCK)
    pad = nb * MOBA_BLOCK - S
    padw = ((0, 0), (0, 0), (0, pad), (0, 0))
    kb = jnp.pad(k, padw).reshape(B, H, nb, MOBA_BLOCK, HEAD_DIM)
    vb = jnp.pad(v, padw).reshape(B, H, nb, MOBA_BLOCK, HEAD_DIM)
    k_mean = jnp.mean(kb, axis=3)

    q_blk = jnp.arange(S) // MOBA_BLOCK
    past = jnp.arange(nb)[None, :] < q_blk[:, None]
    gate = jnp.einsum('bhsd,bhnd->bhsn', q, k_mean).astype(jnp.float32)
    gate = jnp.where(past[None, None], gate, -jnp.inf)
    topk = min(MOBA_TOPK, nb)
    g_vals, g_idx = lax.top_k(gate, topk)
    g_valid = jnp.isfinite(g_vals)

    scale = HEAD_DIM ** -0.5
    nq = S // Q_BLOCK
    bi = jnp.arange(B)[:, None, None, None]
    hi = jnp.arange(H)[None, :, None, None]
    sel_len = topk * MOBA_BLOCK

    def block_fn(args):
        q_c, idx_c, valid_c, c = args
        q0 = c * Q_BLOCK
        own = q0 // MOBA_BLOCK
        k_sel = kb[bi, hi, idx_c]
        v_sel = vb[bi, hi, idx_c]
        s_sel = jnp.einsum('bhqd,bhqjkd->bhqjk', q_c, k_sel).astype(jnp.float32) * scale
        s_sel = jnp.where(valid_c[..., None], s_sel, -jnp.inf).reshape(B, H, Q_BLOCK, sel_len)
        k_own = lax.dynamic_index_in_dim(kb, own, axis=2, keepdims=False)
        v_own = lax.dynamic_index_in_dim(vb, own, axis=2, keepdims=False)
        s_own = jnp.einsum('bhqd,bhkd->bhqk', q_c, k_own).astype(jnp.float32) * scale
        q_pos = q0 + jnp.arange(Q_BLOCK)
        k_pos = own * MOBA_BLOCK + jnp.arange(MOBA_BLOCK)
        s_own = jnp.where(k_pos[None, :] <= q_pos[:, None], s_own, -jnp.inf)
        p = jax.nn.softmax(jnp.concatenate([s_sel, s_own], axis=-1), axis=-1)
        p_sel = p[..., :sel_len].reshape(B, H, Q_BLOCK, topk, MOBA_BLOCK).astype(v.dtype)
        p_own = p[..., sel_len:].astype(v.dtype)
        return (jnp.einsum('bhqjk,bhqjkd->bhqd', p_sel, v_sel)
                + jnp.einsum('bhqk,bhkd->bhqd', p_own, v_own))

    outs = lax.map(block_fn, (to_qblocks(q), to_qblocks(g_idx), to_qblocks(g_valid),
                              jnp.arange(nq)))
    out = jnp.moveaxis(outs, 0, 2).reshape(B, H, S, HEAD_DIM)
    return out.transpose(0, 2, 1, 3).reshape(B, S, WIDTH_B)


def setup_inputs(seed: int = 0) -> dict:
    key = jax.random.key(seed)
    ks = jax.random.split(key, 17)
    f32 = jnp.float32
    nrm = lambda k, shape, s: jax.random.normal(k, shape, f32) * s
    gain = lambda k, n: 1.0 + 0.02 * jax.random.normal(k, (DEPTH, n), f32)
    return {
        "x": jax.random.normal(ks[0], (BATCH, SEQ, D_MODEL), f32),
        "norm1_g": gain(ks[1], D_MODEL),
        "w_in": nrm(ks[2], (DEPTH, D_MODEL, IN_WIDTH), D_MODEL ** -0.5),
        "sgu_ln_g": gain(ks[3], WIDTH_A),
        "sgu_ln_b": nrm(ks[4], (DEPTH, WIDTH_A), 0.02),
        "sgu_w": nrm(ks[5], (DEPTH, N_HEADS_A, CHUNK, CHUNK), CHUNK ** -0.5),
        "sgu_b": 1.0 + nrm(ks[6], (DEPTH, N_HEADS_A, CHUNK), 0.02),
        "q_norm_g": gain(ks[7], HEAD_DIM),
        "k_norm_g": gain(ks[8], HEAD_DIM),
        "out_norm_a_g": gain(ks[9], WIDTH_A),
        "out_norm_b_g": gain(ks[10], WIDTH_B),
        "w_out": nrm(ks[11], (DEPTH, MIX_WIDTH, D_MODEL), MIX_WIDTH ** -0.5),
        "norm2_g": gain(ks[12], D_MODEL),
        "w_gate": nrm(ks[13], (DEPTH, D_MODEL, D_FF), D_MODEL ** -0.5),
        "w_up": nrm(ks[14], (DEPTH, D_MODEL, D_FF), D_MODEL ** -0.5),
        "w_down": nrm(ks[15], (DEPTH, D_FF, D_MODEL), D_FF ** -0.5),
    }


def reference(x, norm1_g, w_in, sgu_ln_g, sgu_ln_b, sgu_w, sgu_b, q_norm_g, k_norm_g,
              out_norm_a_g, out_norm_b_g, w_out, norm2_g, w_gate, w_up, w_down):
    B, S, _ = x.shape
    for l in range(DEPTH):
        h = rms_norm(x, norm1_g[l])
        proj = h @ w_in[l]
        u_a, v_a, q_b, k_b, v_b = jnp.split(
            proj, [WIDTH_A, 2 * WIDTH_A, 2 * WIDTH_A + WIDTH_B, 2 * WIDTH_A + 2 * WIDTH_B], axis=-1)
        y_a = chunked_sgu(u_a, v_a, sgu_ln_g[l], sgu_ln_b[l], sgu_w[l], sgu_b[l])
        y_b = moba_attention(q_b, k_b, v_b, q_norm_g[l], k_norm_g[l])
        y_a = rms_norm(y_a.reshape(B, S, N_HEADS_A, HEAD_DIM),
                       out_norm_a_g[l].reshape(N_HEADS_A, HEAD_DIM)).reshape(B, S, WIDTH_A)
        y_b = rms_norm(y_b.reshape(B, S, N_HEADS_B, HEAD_DIM),
                       out_norm_b_g[l].reshape(N_HEADS_B, HEAD_DIM)).reshape(B, S, WIDTH_B)
        x = x + jnp.concatenate([y_a, y_b], axis=-1) @ w_out[l]
        h = rms_norm(x, norm2_g[l])
        x = x + (jax.nn.silu(h @ w_gate[l]) * (h @ w_up[l])) @ w_down[l]
    return x
```

```python
import numpy as np
from contextlib import ExitStack

import concourse.bass as bass
import concourse.mybir as mybir
from concourse.bass_utils import run_bass_kernel_spmd

F32 = mybir.dt.float32
BF16 = mybir.dt.bfloat16
AF = mybir.ActivationFunctionType
ALU = mybir.AluOpType
AX = mybir.AxisListType

S_LEN = 2048
D = 1024
NT = 16
H = 8
HD = 64
DFF = 2816
NF = 22
EPS = 1e-6
NEG = -30000.0


class _Op:
    __slots__ = ("idx", "eng", "fn", "deps", "is_dma", "dkey", "signal", "ticket", "total")

    def __init__(self, idx, eng, fn, is_dma, dkey, total):
        self.idx = idx
        self.eng = eng
        self.fn = fn
        self.deps = []
        self.is_dma = is_dma
        self.dkey = dkey
        self.signal = False
        self.ticket = None
        self.total = total


class Sched:
    ENGS = ("pe", "act", "dve", "pool", "sp")

    def __init__(self):
        self.ops = []
        self.last_writer = {}
        self.readers = {}
        self.dkeys = []

    def add(self, eng, fn, r=(), w=(), dma=None, total=False, after_all=False):
        op = _Op(len(self.ops), eng, fn, dma is not None, dma, total)
        if dma is not None and dma not in self.dkeys:
            self.dkeys.append(dma)
        deps = {}
        if after_all:
            last = {}
            for o in self.ops:
                if not o.is_dma:
                    last[o.eng] = o
            for o in last.values():
                if o.eng != eng:
                    deps[o.idx] = o
        for k in r:
            x = self.last_writer.get(k)
            if x is not None:
                deps[x.idx] = x
        for k in w:
            x = self.last_writer.get(k)
            if x is not None:
                deps[x.idx] = x
            for rd in self.readers.get(k, ()):
                deps[rd.idx] = rd
        for k in r:
            self.readers.setdefault(k, []).append(op)
        for k in w:
            self.last_writer[k] = op
            self.readers[k] = []
        for d in deps.values():
            if d.eng == "pe" and eng == "pe" and not d.is_dma and dma is None:
                continue
            op.deps.append(d)
            d.signal = True
        self.ops.append(op)
        return op

    def assign(self, sems, dma_sems):
        cnt = {e: 0 for e in self.ENGS}
        dcnt = {}
        for op in self.ops:
            if op.is_dma:
                dcnt[op.dkey] = dcnt.get(op.dkey, 0) + 16
                op.ticket = (dma_sems[op.dkey], dcnt[op.dkey])
            elif op.signal:
                cnt[op.eng] += 1
                op.ticket = (sems[op.eng], cnt[op.eng])
        for op in self.ops:
            if op.is_dma and op.total:
                op.ticket = (dma_sems[op.dkey], dcnt[op.dkey])
        self.dtotals = {k: (dma_sems[k], v) for k, v in dcnt.items()}

    def run_engine(self, name, eng, final_waits=()):
        waited = {}
        for op in self.ops:
            if op.eng != name:
                continue
            for d in op.deps:
                sem, val = d.ticket
                if waited.get(id(sem), 0) >= val:
                    continue
                eng.wait_ge(sem, val)
                waited[id(sem)] = val
            ins = op.fn(eng)
            if op.is_dma:
                ins.then_inc(op.ticket[0], 16)
            elif op.signal:
                ins.then_inc(op.ticket[0], 1)
        for sem, val in final_waits:
            eng.wait_ge(sem, val)


def build_nc(debug=False, dbg_tile=0):
    nc = bass.Bass("TRN2", target_bir_lowering=False)
    dram = nc.dram_tensor
    x_d = dram("x", [S_LEN, D], F32, kind="ExternalInput").ap()
    win_d = dram("w_in_r", [128, 8 * 2560], F32, kind="ExternalInput").ap()
    wout_d = dram("w_out_r", [128, 8 * 1024], F32, kind="ExternalInput").ap()
    wg_d = dram("wg_r", [NF, 128, 1024], F32, kind="ExternalInput").ap()
    wu_d = dram("wu_r", [NF, 128, 1024], F32, kind="ExternalInput").ap()
    wd_d = dram("wd_r", [NF, 128, 1024], F32, kind="ExternalInput").ap()
    sgw_d = dram("sgu_wT", [128, 1024], F32, kind="ExternalInput").ap()
    sgb_d = dram("sgu_bT", [128, 8], F32, kind="ExternalInput").ap()
    rowc_d = dram("rowc", [128, 1152], F32, kind="ExternalInput").ap()
    grow_d = dram("grows", [128, 3072], F32, kind="ExternalInput").ap()
    gcol_d = dram("gcols", [128, 16], F32, kind="ExternalInput").ap()
    out_d = dram("out", [S_LEN, D], F32, kind="ExternalOutput").ap()
    x1_d = dram("x1s", [S_LEN, D], F32, kind="ExternalOutput" if debug else "Internal").ap()
    dbg = {}
    if debug:
        for nm, shp, dt_ in (("d_u", [128, 512], F32), ("d_vln", [128, 512], BF16), ("d_qaug", [128, 512], BF16),
                             ("d_kaug", [128, 576], BF16), ("d_ycat", [128, 1024], BF16), ("d_hb", [128, 1024], BF16),
                             ("d_ob", [128, 512], F32), ("d_qT", [128, 1024], BF16), ("d_kT", [128, 1024], BF16)):
            dbg[nm] = dram(nm, shp, dt_, kind="ExternalOutput").ap()

    S = Sched()
    A = S.add

    with ExitStack() as es:
        def sb(name, shape, dt=F32):
            return es.enter_context(nc.sbuf_tensor(name, shape, dt))

        def ps(name, shape, dt=F32):
            return es.enter_context(nc.psum_tensor(name, shape, dt))

        arena = sb("arena", [128, 53376], BF16)
        ar = arena[:]
        win = ar[:, 0:20480].rearrange("p (c n) -> p c n", c=8)
        wout = ar[:, 20480:28672].rearrange("p (c n) -> p c n", c=8)
        kT = ar[:, 28672:45056].rearrange("p (h t) -> p h t", h=8)
        Vv = ar[:, 45056:53376].rearrange("p (t h d) -> p t h d", t=16, h=8)
        h2T = ar[:, 0:8192].rearrange("p (c t) -> p c t", c=8)
        aT = ar[:, 8192:30720].rearrange("p (f t) -> p f t", f=NF)
        wdb = ar[:, 30720:53248].rearrange("p (f n) -> p f n", f=NF)

        NST = 3
        stage = [sb(f"stage{k}", [128, 1024]) for k in range(NST)]
        NXT = 4
        xt = [sb(f"xt{k}", [128, 1024]) for k in range(NXT)]
        hb = sb("hb", [128, 1024], BF16)
        hT = [sb(f"hT{k}", [128, 8, 128], BF16) for k in range(2)]
        u_sbs = [sb(f"u_sb{k}", [128, 512]) for k in range(2)]
        vg = sb("vg", [128, 512])
        sq = sb("sq", [128, 512])
        sqq = sb("sqq", [128, 512])
        sqk = sb("sqk", [128, 512])
        vlns = [sb(f"vln{k}", [128, 512], BF16) for k in range(2)]
        qf = sb("qf", [128, 512])
        kf = sb("kf", [128, 512])
        qaug = [sb(f"qaug{k}", [128, 8, 64], BF16) for k in range(2)]
        qmask = [sb(f"qmask{k}", [128, 8, 8], BF16) for k in range(2)]
        kaug = [sb(f"kaug{k}", [128, 8, 72], BF16) for k in range(2)]
        qT = [sb(f"qT{k}", [128, 8, 128], BF16) for k in range(2)]
        NPT = 3
        PT = [sb(f"PT{k}", [128, 512], BF16) for k in range(NPT)]
        ob = sb("ob", [128, 512])
        ya = sb("ya", [128, 512])
        identf = ya[:, 0:128]
        trif = ya[:, 128:256]
        causal = ya[:, 256:384]
        ycat = sb("ycat", [128, 1024], BF16)
        yT = sb("yT", [128, 8, 128], BF16)
        x1t = sb("x1t", [128, 1024])
        grow = sb("grow", [128, 1024])
        gcols = sb("gcols_sb", [128, 16])
        rowc = sb("rowc_sb", [128, 1152])
        sgb = sb("sgb", [128, 8])
        wsg = sb("wsg", [128, 8, 128], BF16)
        ident = sb("ident", [128, 128], BF16)
        trimask = sb("trimask", [128, 128], BF16)
        mhalf = sb("mhalf", [128, 8])
        onescol = sb("onescol", [128, 1], BF16)
        gate_sb = sb("gate_sb", [128, 8, 8])
        top8 = sb("top8", [128, 8, 8])
        sel = sb("sel", [128, 8, 8])
        kmT = sb("kmT", [64, 8, 8], BF16)
        kmpart = sb("kmpart", [64, 8])
        st = {n: sb("st_" + n, [128, 8]) for n in
              ("ss1", "rs1", "vsum", "vneg", "vss", "vrs", "qss", "qrs", "kss", "krs",
               "ass", "ars", "bss", "brs", "osum", "ss2", "rs2")}
        wgb = [hT[0], hT[1]]
        wub = [qT[0], qT[1]]
        sgs = [qf, kf]
        ot = x1t

        G = [ps(f"G{k}", [128, 512]) for k in range(3)]
        SB = [ps(f"S{k}", [128, 512]) for k in range(3)]

        def TB(gb):
            return G[gb][:, :].bitcast(BF16)
        O = [ps(f"O{k}", [128, 512]) for k in range(2)]

        g2row = grow[:, 0:1024]
        lng = rowc[:, 0:512]
        lnb = rowc[:, 512:1024]
        qg = rowc[:, 1024:1088]
        kg = rowc[:, 1088:1152]

        cnt = {"g": 0, "t": 0, "st": 0, "cast": 0}

        open_g = set()

        def nextG():
            for _ in range(len(G)):
                cnt["g"] += 1
                gb = (cnt["g"] - 1) % len(G)
                if gb not in open_g:
                    return gb
            raise RuntimeError("all general PSUM banks hold open accumulation groups")

        def nextT():
            cnt["t"] += 1
            return (cnt["t"] - 1) % 2

        def v3(ap, h=8):
            return ap.rearrange("p (h d) -> p h d", h=h)

        def bc(ap, n):
            return ap.unsqueeze(2).to_broadcast([ap.shape[0], ap.shape[1], n])

        slots = {"list": [(stage[k], [f"stage{k}"], f"stage{k}") for k in range(NST)] + [(x1t, ["x1t0", "x1t1"], "x1t"), (xt[3], ["xt3"], "xt3")]}

        def load_cast(src_ap, dst_ap, width, dst_key, eng=None, rkeys=(), gain_cols=None):
            rkeys = list(rkeys)
            dkeys_ = list(dst_key) if isinstance(dst_key, (list, tuple)) else [dst_key]
            sl_list = slots["list"]
            tns, keys, dkey = sl_list[cnt["st"] % len(sl_list)]
            cnt["st"] += 1
            dst_stage = tns[:, 0:width]
            if len(src_ap.shape) == 3:
                dst_stage = dst_stage.rearrange("p (c n) -> p c n", c=src_ap.shape[1])
            A("sp", lambda e: e.dma_start(out=dst_stage, in_=src_ap), w=keys, dma=dkey)
            if eng is None:
                eng = "pool"
            src = tns[:, 0:width]
            if len(dst_ap.shape) == 3:
                src = src.rearrange("p (c n) -> p c n", c=dst_ap.shape[1])
            if gain_cols is not None:
                nchunk = len(gain_cols)
                wch = width // nchunk
                for j, gc in enumerate(gain_cols):
                    d_ap = dst_ap[:, j, :] if len(dst_ap.shape) == 3 else dst_ap
                    s_ap = tns[:, j * wch:(j + 1) * wch]
                    gap = gcols[:, gc:gc + 1]
                    if eng == "act":
                        A("act", lambda e, d_ap=d_ap, s_ap=s_ap, gap=gap: e.activation(out=d_ap, in_=s_ap, func=AF.Copy, scale=gap),
                          r=keys + rkeys + ["gcols"], w=[f"{dkeys_[0]}#{j}"])
                    else:
                        A(eng, lambda e, d_ap=d_ap, s_ap=s_ap, gap=gap: e.tensor_scalar(out=d_ap, in0=s_ap, scalar1=gap, scalar2=None,
                                                                                         op0=ALU.mult),
                          r=keys + rkeys + ["gcols"], w=[f"{dkeys_[0]}#{j}"])
            elif eng == "act":
                A("act", lambda e: e.activation(out=dst_ap, in_=src, func=AF.Copy), r=keys + rkeys, w=dkeys_)
            else:
                A(eng, lambda e: e.tensor_copy(out=dst_ap, in_=src), r=keys + rkeys, w=dkeys_)

        def rstd_chain(ss, rs, n, cols=8):
            A("pool", lambda e: e.tensor_scalar(out=st[rs][:, 0:cols], in0=st[ss][:, 0:cols], scalar1=1.0 / n, scalar2=EPS,
                                                op0=ALU.mult, op1=ALU.add), r=[ss], w=[rs])
            A("pool", lambda e: e.tensor_tensor(out=st[rs][:, 0:cols], in0=st[rs][:, 0:cols], in1=mhalf[:, 0:cols], op=ALU.pow),
              r=[rs, "mhalf"], w=[rs])

        def rstd_steps(ss, rs, n, cols=8):
            return [
                lambda: A("pool", lambda e: e.tensor_scalar(out=st[rs][:, 0:cols], in0=st[ss][:, 0:cols], scalar1=1.0 / n, scalar2=EPS,
                                                            op0=ALU.mult, op1=ALU.add), r=[ss], w=[rs]),
                lambda: A("pool", lambda e: e.tensor_tensor(out=st[rs][:, 0:cols], in0=st[rs][:, 0:cols], in1=mhalf[:, 0:cols], op=ALU.pow),
                          r=[rs, "mhalf"], w=[rs]),
            ]

        chains = []

        def defer(name, steps):
            chains.append({"name": name, "steps": list(steps)})

        def pump():
            for ch in list(chains):
                if ch["steps"]:
                    ch["steps"].pop(0)()
                if not ch["steps"]:
                    chains.remove(ch)

        def flush(name=None):
            for ch in list(chains):
                if name is None or ch["name"] == name:
                    while ch["steps"]:
                        ch["steps"].pop(0)()
                    chains.remove(ch)

        A("pool", lambda e: e.memset(identf, 1.0), w=["identf"])
        A("pool", lambda e: e.affine_select(out=identf, in_=identf, pattern=[[-1, 128]], compare_op=ALU.is_equal,
                                            fill=0.0, base=0, channel_multiplier=1), r=["identf"], w=["identf"])
        A("pool", lambda e: e.tensor_copy(out=ident[:], in_=identf), r=["identf"], w=["ident"])
        A("pool", lambda e: e.memset(causal, 1.0), w=["causal"])
        A("pool", lambda e: e.affine_select(out=causal, in_=causal, pattern=[[1, 128]], compare_op=ALU.is_ge,
                                            fill=0.0, base=0, channel_multiplier=-1), r=["causal"], w=["causal"])
        A("pool", lambda e: e.memset(trif, 0.0), w=["trif"])
        A("pool", lambda e: e.affine_select(out=trif, in_=trif, pattern=[[1, 128]], compare_op=ALU.is_ge,
                                            fill=NEG, base=0, channel_multiplier=-1), r=["trif"], w=["trif"])
        A("pool", lambda e: e.tensor_copy(out=trimask[:], in_=trif), r=["trif"], w=["trimask"])
        A("pool", lambda e: e.memset(mhalf[:], -0.5), w=["mhalf"])
        A("pool", lambda e: e.memset(onescol[:], 1.0 / 256.0), w=["onescol"])
        A("pool", lambda e: e.memset(gate_sb[:], -1e30), w=["gate_sb"])
        A("pool", lambda e: e.memset(kmT[:], 0.0), w=["kmT"])
        for k in range(2):
            A("pool", lambda e, k=k: e.memset(qmask[k][:], 0.0), w=[f"qmask{k}"])
            A("pool", lambda e, k=k: e.memset(qT[k][64:72, :, :], 0.0), w=[f"qTm{k}"])
        A("pool", lambda e: e.memset(Vv[:, :, :, 64:65], 1.0), w=["Vones"])

        A("sp", lambda e: e.dma_start(out=xt[0][:], in_=x_d[0:128, :]), w=["xt0"], dma="xt0")
        A("sp", lambda e: e.dma_start(out=gcols[:], in_=gcol_d[:, :]), w=["gcols"], dma="const", total=True)
        A("sp", lambda e: e.dma_start(out=xt[1][:], in_=x_d[128:256, :]), w=["xt1"], dma="xt1")
        A("sp", lambda e: e.dma_start(out=rowc[:], in_=rowc_d[:, :]), w=["rowc"], dma="const", total=True)
        A("sp", lambda e: e.dma_start(out=sgb[:], in_=sgb_d[:, :]), w=["sgb"], dma="const", total=True)
        A("pool", lambda e: e.tensor_tensor(out=kg, in0=kg, in1=qg, op=ALU.mult), r=["rowc"], w=["rowc"])

        def load_x(i):
            k = i % NXT
            A("sp", lambda e: e.dma_start(out=xt[k][:], in_=x_d[i * 128:(i + 1) * 128, :]), w=[f"xt{k}"], dma=f"xt{k}")

        WOUTKEYS = [f"wout{c}#0" for c in range(8)]
        win_d3 = win_d.rearrange("p (c n) -> p c n", c=8)
        pcnt = {"n": 0}

        def win_pieces(sl):
            for cp in range(4):
                key = f"win{sl}_{cp}"
                load_cast(win_d3[:, 2 * cp:2 * cp + 2, sl * 512:(sl + 1) * 512], win[:, 2 * cp:2 * cp + 2, sl * 512:(sl + 1) * 512],
                          1024, key, eng=("dve", "act")[pcnt["n"] % 2], gain_cols=[2 * cp, 2 * cp + 1])
                pcnt["n"] += 1
                pump()


        def stage_A0a(i, deferred=True):
            k = i % NXT
            xk = f"xt{k}"
            steps = [lambda: A("act", lambda e: e.activation(out=hb[:], in_=xt[k][:], func=AF.Square, accum_out=st["ss1"][:, 0:1]),
                               r=[xk], w=["hb", "ss1"])]
            steps += rstd_steps("ss1", "rs1", 1024.0, cols=1)
            steps.append(lambda: A("dve", lambda e: e.tensor_scalar(out=hb[:], in0=xt[k][:], scalar1=st["rs1"][:, 0:1], scalar2=None,
                                                                    op0=ALU.mult), r=[xk, "rs1"], w=["hb"]))
            defer("a0", steps)
            if not deferred:
                flush("a0")

        def stage_A0b(i):
            s2 = i % 2
            flush("a0")

            tb = nextG()

            def tr(e):
                for c in range(8):
                    ins = e.transpose(out=TB(tb)[:, c * 128:(c + 1) * 128], in_=hb[:, c * 128:(c + 1) * 128], identity=ident[:])
                return ins
            A("pe", tr, r=["hb", "ident"], w=[f"G{tb}"])
            A("dve", lambda e: e.tensor_copy(out=hT[s2][:].rearrange("p c t -> p (c t)"), in_=TB(tb)[:, :]), r=[f"G{tb}"], w=[f"hT{s2}"])

        def stage_A0(i):
            stage_A0a(i, deferred=False)
            stage_A0b(i)

        def A1_parts(i):
            s2 = i % 2
            parts = []

            def proj_half(sl, gb, half):
                def f(e):
                    for c in range(4 * half, 4 * half + 4):
                        ins = e.matmul(G[gb][:, :], lhsT=hT[s2][:, c, :], rhs=win[:, c, sl * 512:(sl + 1) * 512],
                                       start=(c == 0), stop=(c == 7))
                    return ins
                return lambda: A("pe", f, r=[f"hT{s2}"] + [f"win{sl}_{2 * half + a_}#{b_}" for a_ in range(2) for b_ in range(2)],
                                 w=[f"G{gb}"])

            def qk_slice(nm, sl, src, sqb, ssn, rsn, gvec, dst, dkey):
                gbox = {}

                def first():
                    gbox["gb"] = nextG()
                    open_g.add(gbox["gb"])
                    proj_half(sl, gbox["gb"], 0)()

                def second():
                    gb = gbox["gb"]
                    proj_half(sl, gb, 1)()
                    open_g.discard(gb)
                    A("dve", lambda e: e.tensor_copy(out=src[:], in_=G[gb][:, :]), r=[f"G{gb}"], w=[nm + "f"])
                    steps = [
                        lambda: A("pool", lambda e: e.tensor_tensor(out=sqb[:], in0=src[:], in1=src[:], op=ALU.mult), r=[nm + "f"], w=["sq" + nm]),
                        lambda: A("dve", lambda e: e.tensor_reduce(out=st[ssn][:], in_=v3(sqb[:]), axis=AX.X, op=ALU.add), r=["sq" + nm], w=[ssn]),
                    ] + rstd_steps(ssn, rsn, 64.0)
                    if gvec is None:
                        steps.append(lambda: A("dve", lambda e: e.tensor_tensor(out=dst, in0=v3(src[:]), in1=bc(st[rsn][:], 64), op=ALU.mult),
                                               r=[nm + "f", rsn], w=[dkey]))
                    else:
                        steps.append(lambda: A("dve", lambda e: e.tensor_tensor(out=v3(src[:]), in0=v3(src[:]), in1=bc(st[rsn][:], 64),
                                                                                op=ALU.mult), r=[nm + "f", rsn], w=[nm + "f"]))
                        steps.append(lambda: A("dve", lambda e: e.tensor_tensor(out=dst, in0=v3(src[:]),
                                                                                in1=gvec.unsqueeze(1).to_broadcast([128, 8, 64]),
                                                                                op=ALU.mult), r=[nm + "f", "rowc"], w=[dkey]))
                    defer(nm + "chain", steps)
                return [first, second]

            def gen_slice(sl, evac):
                gbox = {}

                def first():
                    gbox["gb"] = nextG()
                    open_g.add(gbox["gb"])
                    proj_half(sl, gbox["gb"], 0)()

                def second_pe():
                    proj_half(sl, gbox["gb"], 1)()
                    open_g.discard(gbox["gb"])

                def second_evac():
                    evac(gbox["gb"])

                def second():
                    second_pe()
                    second_evac()
                return [first, second, second_pe, second_evac]

            def evac_u(gb):
                A("act", lambda e: e.activation(out=u_sbs[s2][:], in_=G[gb][:, :], func=AF.Gelu_apprx_tanh), r=[f"G{gb}"], w=[f"u{s2}"])

            def evac_va(gb):
                A("act", lambda e: e.activation(out=vg[:], in_=G[gb][:, :], func=AF.Gelu_apprx_tanh), r=[f"G{gb}"], w=["vg"])
                flush("qchain")
                steps = [
                    lambda: A("dve", lambda e: e.tensor_reduce(out=st["vsum"][:], in_=v3(vg[:]), axis=AX.X, op=ALU.add), r=["vg"], w=["vsum"]),
                    lambda: A("pool", lambda e: e.tensor_scalar(out=st["vneg"][:], in0=st["vsum"][:], scalar1=-1.0 / 64, scalar2=None,
                                                                op0=ALU.mult), r=["vsum"], w=["vneg"]),
                    lambda: A("dve", lambda e: e.tensor_tensor(out=v3(vg[:]), in0=v3(vg[:]), in1=bc(st["vneg"][:], 64), op=ALU.add),
                              r=["vg", "vneg"], w=["vg"]),
                    lambda: A("pool", lambda e: e.tensor_tensor(out=sqq[:], in0=vg[:], in1=vg[:], op=ALU.mult), r=["vg"], w=["sqq"]),
                    lambda: A("dve", lambda e: e.tensor_reduce(out=st["vss"][:], in_=v3(sqq[:]), axis=AX.X, op=ALU.add), r=["sqq"], w=["vss"]),
                ] + rstd_steps("vss", "vrs", 64.0) + [
                    lambda: A("dve", lambda e: e.tensor_tensor(out=v3(vg[:]), in0=v3(vg[:]), in1=bc(st["vrs"][:], 64), op=ALU.mult),
                              r=["vg", "vrs"], w=["vg"]),
                    lambda: A("pool", lambda e: e.tensor_tensor(out=vg[:], in0=vg[:], in1=lng, op=ALU.mult), r=["vg", "rowc"], w=["vg"]),
                    lambda: A("dve", lambda e: e.tensor_tensor(out=vlns[s2][:], in0=vg[:], in1=lnb, op=ALU.add), r=["vg", "rowc"], w=[f"vln{s2}"]),
                ]
                defer(f"ln{s2}", steps)

            def evac_vb(gb):
                A("dve", lambda e: e.tensor_copy(out=Vv[:, i, :, 0:64], in_=v3(G[gb][:, :])), r=[f"G{gb}"], w=[f"V{i}"])

            pq = qk_slice("q", 2, qf, sqq, "qss", "qrs", None, qaug[s2][:, :, :], f"qaug{s2}")
            pk = qk_slice("k", 3, kf, sqk, "kss", "krs", kg, kaug[s2][:, :, 0:64], f"kaug{s2}")
            nblk = i // 2

            def k_first():
                A("pool", lambda e: e.memset(kaug[s2][:, :, 64:72], 0.0), w=[f"kaugoh{s2}"])
                A("pool", lambda e: e.memset(kaug[s2][:, :, 64 + nblk:65 + nblk], 1.0), w=[f"kaugoh{s2}"])
                pk[0]()
            pu = gen_slice(0, evac_u)
            pva = gen_slice(1, evac_va)
            pvb = gen_slice(4, evac_vb)

            def uva_pe():
                pu[2]()
                pva[2]()

            def uva_evac():
                pu[3]()
                pva[3]()
            parts = [pq[0], pq[1], k_first, pk[1], pu[0], pva[0], uva_pe, uva_evac, pvb[0], pvb[1]]
            return parts

        def stage_Bk(i):
            s2 = i % 2
            n = i // 2
            flush("kchain")

            tb = nextG()

            def trk(e):
                for h in range(8):
                    ins = e.transpose(out=TB(tb)[0:72, h * 128:(h + 1) * 128], in_=kaug[s2][:, h, :], identity=ident[:])
                return ins
            A("pe", trk, r=[f"kaug{s2}", f"kaugoh{s2}", "ident"], w=[f"G{tb}"])
            A("dve", lambda e: e.tensor_copy(out=kT[0:72, :, i * 128:(i + 1) * 128], in_=v3(TB(tb)[0:72, :])), r=[f"G{tb}"], w=[f"kT{i}"])

            def km(e):
                for h in range(8):
                    ins = e.matmul(O[0][0:64, 300 + h:301 + h], lhsT=kaug[s2][:, h, 0:64], rhs=onescol[:, :], start=True, stop=True)
                return ins
            A("pe", km, r=[f"kaug{s2}", "onescol"], w=["O0misc"])
            if i % 2 == 0:
                A("dve", lambda e: e.tensor_copy(out=kmpart[:], in_=O[0][0:64, 300:308]), r=["O0misc"], w=["kmpart"])
            else:
                A("dve", lambda e: e.tensor_tensor(out=kmT[:, :, n], in0=O[0][0:64, 300:308], in1=kmpart[:], op=ALU.add),
                  r=["O0misc", "kmpart"], w=["kmT"])

        def stage_Q1a(i):
            s2 = i % 2
            flush("qchain")
            tb = nextG()

            def trq(e):
                for h in range(8):
                    ins = e.transpose(out=TB(tb)[0:64, h * 128:(h + 1) * 128], in_=qaug[s2][:, h, :], identity=ident[:])
                return ins
            A("pe", trq, r=[f"qaug{s2}", "ident"], w=[f"G{tb}"])
            A("dve", lambda e: e.tensor_copy(out=qT[s2][0:64, :, :], in_=v3(TB(tb)[0:64, :])), r=[f"G{tb}"], w=[f"qT{s2}"])

        def stage_Q1b(i):
            s2 = i % 2
            qb = i // 2
            if i >= 8:
                def gm(e):
                    for h in range(8):
                        ins = e.matmul(O[0][:, 320 + h * 8:320 + h * 8 + qb], lhsT=qT[s2][0:64, h, :], rhs=kmT[:, h, 0:qb],
                                       start=True, stop=True)
                    return ins
                A("pe", gm, r=[f"qT{s2}", "kmT"], w=["O0misc"])
                A("dve", lambda e: e.tensor_copy(out=gate_sb[:, :, 0:qb], in_=v3(O[0][:, 320:384])[:, :, 0:qb]), r=["O0misc"], w=["gate_sb"])
                for h in range(8):
                    A("dve", lambda e, h=h: e.max(out=top8[:, h, :], in_=gate_sb[:, h, :]), r=["gate_sb"], w=[f"top8_{h}"])
                A("dve", lambda e: e.tensor_tensor(out=sel[:, :, 0:qb], in0=gate_sb[:, :, 0:qb],
                                                   in1=top8[:, :, 2:3].to_broadcast([128, 8, qb]), op=ALU.is_ge),
                  r=["gate_sb"] + [f"top8_{h}" for h in range(8)], w=["sel"])
                A("dve", lambda e: e.tensor_scalar(out=qmask[s2][:, :, 0:qb], in0=sel[:, :, 0:qb], scalar1=1.0, scalar2=-NEG,
                                                   op0=ALU.subtract, op1=ALU.mult), r=["sel"], w=[f"qmask{s2}"])

        def stage_Q1(i):
            stage_Q1a(i)
            stage_Q1b(i)

        def stage_Q2(i):
            s2 = i % 2
            if i >= 8:
                tb = nextG()

                def trm(e):
                    for h in range(8):
                        ins = e.transpose(out=TB(tb)[64:72, h * 128:(h + 1) * 128], in_=qmask[s2][:, h, :], identity=ident[:])
                    return ins
                A("pe", trm, r=[f"qmask{s2}", "ident"], w=[f"G{tb}"])
                A("dve", lambda e: e.tensor_copy(out=qT[s2][64:72, :, :], in_=v3(TB(tb)[64:72, :])), r=[f"G{tb}"], w=[f"qTm{s2}"])

        def stage_SGU(i):
            s2 = i % 2
            flush(f"ln{s2}")
            gb = nextG()

            def mix(e):
                for h in range(8):
                    ins = e.matmul(G[gb][:, h * 64:(h + 1) * 64], lhsT=wsg[:, h, :], rhs=vlns[s2][:, h * 64:(h + 1) * 64],
                                   start=True, stop=True)
                return ins
            A("pe", mix, r=["wsg", f"vln{s2}"], w=[f"G{gb}"])
            A("dve", lambda e: e.tensor_tensor(out=v3(ya[:]), in0=v3(G[gb][:, :]), in1=bc(sgb[:], 64), op=ALU.add),
              r=[f"G{gb}", "sgb"], w=["ya", "identf", "trif", "causal"])
            flush("kchain")
            steps = [
                lambda: A("dve", lambda e: e.tensor_tensor(out=ya[:], in0=ya[:], in1=u_sbs[s2][:], op=ALU.mult), r=["ya", f"u{s2}"], w=["ya"]),
                lambda: A("pool", lambda e: e.tensor_tensor(out=sqk[:], in0=ya[:], in1=ya[:], op=ALU.mult), r=["ya"], w=["sqk"]),
                lambda: A("dve", lambda e: e.tensor_reduce(out=st["ass"][:], in_=v3(sqk[:]), axis=AX.X, op=ALU.add), r=["sqk"], w=["ass"]),
            ] + rstd_steps("ass", "ars", 64.0) + [
                lambda: A("dve", lambda e: e.tensor_tensor(out=v3(ycat[:, 0:512]), in0=v3(ya[:]), in1=bc(st["ars"][:], 64), op=ALU.mult),
                          r=["ya", "ars"], w=["ycatA"]),
            ]
            defer("sgu", steps)

        def stage_C(i, inserts):
            s2 = i % 2
            ng = (i + 4) // 4
            units = [(h, g) for h in range(8) for g in range(ng)]
            NS = len(SB)

            def emit_qk(u):
                h, g = units[u]
                sbk = u % NS
                js = list(range(4 * g, min(4 * g + 4, i + 1)))

                def f(e):
                    for jj, j in enumerate(js):
                        diag = (j == i)
                        ins = e.matmul(SB[sbk][:, jj * 128:(jj + 1) * 128], lhsT=kT[0:72, h, j * 128:(j + 1) * 128],
                                       rhs=qT[s2][0:72, h, :], start=True, stop=not diag)
                        if diag:
                            ins = e.matmul(SB[sbk][:, jj * 128:(jj + 1) * 128], lhsT=ident[:, :], rhs=trimask[:, :],
                                           start=False, stop=True)
                    return ins
                A("pe", f, r=[f"kT{j}" for j in js] + [f"qT{s2}", f"qTm{s2}", "ident", "trimask"], w=[f"S{sbk}"])

            def emit_exp_pv(u):
                h, g = units[u]
                sbk = u % NS
                pk = u % NPT
                js = list(range(4 * g, min(4 * g + 4, i + 1)))
                wdt = len(js) * 128
                A("act", lambda e: e.activation(out=PT[pk][:, 0:wdt], in_=SB[sbk][:, 0:wdt], func=AF.Exp, scale=0.125),
                  r=[f"S{sbk}"], w=[f"PT{pk}"])
                ohalf = h // 4
                hh = h % 4

                def f(e):
                    for jj, j in enumerate(js):
                        ins = e.matmul(O[ohalf][:, hh * 65:(hh + 1) * 65], lhsT=PT[pk][:, jj * 128:(jj + 1) * 128],
                                       rhs=Vv[:, j, h, :], start=(j == 0), stop=(j == i))
                    return ins
                wk = [f"O{ohalf}"] + (["O0misc"] if ohalf == 0 else [])
                A("pe", f, r=[f"PT{pk}", "Vones"] + [f"V{j}" for j in js], w=wk)

            def emit_oevac(ohalf):
                ov = O[ohalf][:, 0:260].rearrange("p (h d) -> p h d", h=4)
                A("dve", lambda e: e.reciprocal(out=st["osum"][:, ohalf * 4:(ohalf + 1) * 4], in_=ov[:, :, 64]),
                  r=[f"O{ohalf}"], w=[f"osum{ohalf}"])
                A("dve", lambda e: e.tensor_tensor(out=v3(ob[:, ohalf * 256:(ohalf + 1) * 256], h=4), in0=ov[:, :, 0:64],
                                                   in1=bc(st["osum"][:, ohalf * 4:(ohalf + 1) * 4], 64), op=ALU.mult),
                  r=[f"O{ohalf}", f"osum{ohalf}"], w=[f"ob{ohalf}"])

            ins_by_pos = {}
            for pos, fn in inserts:
                ins_by_pos.setdefault(min(pos, len(units) - 1), []).append(fn)
            AHEAD = NS - 1
            for u in range(min(AHEAD, len(units))):
                emit_qk(u)
            for u in range(len(units)):
                if u + AHEAD < len(units):
                    emit_qk(u + AHEAD)
                emit_exp_pv(u)
                h, g = units[u]
                if g == ng - 1 and h == 3:
                    flush("tail")
                    emit_oevac(0)
                if g == ng - 1 and h == 7:
                    emit_oevac(1)
                for fn in ins_by_pos.get(u, ()):
                    fn()
                pump()
                if len(units) <= 16:
                    pump()
            flush("ln0")
            flush("ln1")
            flush("sgu")
            steps = [
                lambda: A("pool", lambda e: e.tensor_tensor(out=sq[:], in0=ob[:], in1=ob[:], op=ALU.mult), r=["ob0", "ob1"], w=["sq"]),
                lambda: A("dve", lambda e: e.tensor_reduce(out=st["bss"][:], in_=v3(sq[:]), axis=AX.X, op=ALU.add), r=["sq"], w=["bss"]),
            ] + rstd_steps("bss", "brs", 64.0) + [
                lambda: A("dve", lambda e: e.tensor_tensor(out=v3(ycat[:, 512:1024]), in0=v3(ob[:]), in1=bc(st["brs"][:], 64), op=ALU.mult),
                          r=["ob0", "ob1", "brs"], w=["ycatB"]),
            ]
            defer("tail", steps)

        def stage_Da(i):
            flush("tail")
            flush("sgu")
            tb = nextG()

            def tr(e):
                for c in range(8):
                    ins = e.transpose(out=TB(tb)[:, c * 128:(c + 1) * 128], in_=ycat[:, c * 128:(c + 1) * 128], identity=ident[:])
                return ins
            A("pe", tr, r=["ycatA", "ycatB", "ident"], w=[f"G{tb}"])
            A("dve", lambda e: e.tensor_copy(out=yT[:].rearrange("p c t -> p (c t)"), in_=TB(tb)[:, :]), r=[f"G{tb}"], w=["yT"])

        def stage_Db(i):
            k = i % NXT
            for half in range(2):
                gb = nextG()

                def f(e, half=half, gb=gb):
                    for c in range(8):
                        ins = e.matmul(G[gb][:, :], lhsT=yT[:, c, :], rhs=wout[:, c, half * 512:(half + 1) * 512],
                                       start=(c == 0), stop=(c == 7))
                    return ins
                A("pe", f, r=["yT"] + WOUTKEYS, w=[f"G{gb}"])
                A("dve", lambda e, half=half, gb=gb: e.tensor_tensor(out=x1t[:, half * 512:(half + 1) * 512], in0=G[gb][:, :],
                                                                     in1=xt[k][:, half * 512:(half + 1) * 512], op=ALU.add),
                  r=[f"G{gb}", f"xt{k}"], w=[f"x1t{half}"])
            A("sp", lambda e: e.dma_start(out=x1_d[i * 128:(i + 1) * 128, :], in_=x1t[:]), r=["x1t0", "x1t1"], w=[f"x1d{i}"], dma="x1t")

        def stage_D(i):
            stage_Da(i)
            stage_Db(i)

        WINKEYS = [f"win{sl}_{cp}#{b_}" for sl in range(5) for cp in range(4) for b_ in range(2)]
        LOOPKEYS = WINKEYS + WOUTKEYS + ["Vones"] + [f"kT{j}" for j in range(NT)] + [f"V{j}" for j in range(NT)]
        hbs_by_half = [
            [(hb[:], ["hb"]), (hT[1][:].rearrange("p c t -> p (c t)"), ["hT1"])],
            [(hb[:], ["hb"]), (ycat[:], ["ycatA", "ycatB"])],
        ]

        def ffn_f1_pre(hf, tt):
            t = hf * 8 + tt
            xs = (t % 3) if hf == 0 else (t + 2) % NXT
            hbk = t % 2
            hbuf, hkeys = hbs_by_half[hf][hbk]
            A("sp", lambda e: e.dma_start(out=xt[xs][:], in_=x1_d[t * 128:(t + 1) * 128, :]),
              r=[f"x1d{t}"], w=[f"xt{xs}"], dma=f"xt{xs}")
            A("act", lambda e: e.activation(out=hbuf, in_=xt[xs][:], func=AF.Square, accum_out=st["ss2"][:, 0:1]),
              r=[f"xt{xs}"], w=["ss2", f"hbf{hbk}"] + hkeys)
            rstd_chain("ss2", "rs2", 1024.0, cols=1)
            A("dve", lambda e: e.scalar_tensor_tensor(out=hbuf, in0=xt[xs][:], scalar=st["rs2"][:, 0:1], in1=g2row,
                                                      op0=ALU.mult, op1=ALU.mult), r=[f"xt{xs}", "rs2", "grow2"],
              w=[f"hbf{hbk}"] + hkeys)

        def ffn_f1_post(hf, tt):
            t = hf * 8 + tt
            hbk = t % 2
            hbuf, hkeys = hbs_by_half[hf][hbk]
            tb = nextG()

            def tr(e):
                for c in range(8):
                    ins = e.transpose(out=TB(tb)[:, c * 128:(c + 1) * 128], in_=hbuf[:, c * 128:(c + 1) * 128], identity=ident[:])
                return ins
            A("pe", tr, r=[f"hbf{hbk}", "ident"], w=[f"G{tb}"])
            A("dve", lambda e: e.tensor_copy(out=h2T[:, :, tt * 128:(tt + 1) * 128], in_=v3(TB(tb)[:, :])),
              r=[f"G{tb}"], w=[f"h2T{tt}"] + WINKEYS)

        def ffn_half(hf):
            ffn_f1_pre(hf, 0)
            for tt in range(8):
                if tt + 1 < 8:
                    ffn_f1_pre(hf, tt + 1)
                ffn_f1_post(hf, tt)

        def ffn_prefetch(hf, f):
            ws = f % 2
            load_cast(wg_d[f, :, :], wgb[ws][:], 1024, [f"wgb{ws}", f"hT{ws}"], eng="act")
            load_cast(wu_d[f, :, :], wub[ws][:], 1024, [f"wub{ws}", f"qT{ws}", f"qTm{ws}"], eng="dve")
            if hf == 0:
                load_cast(wd_d[f, :, :], wdb[:, f, :], 1024, f"wdb{f}", eng="pool", rkeys=["ffnbar"])

        def run(fns):
            for fn in fns:
                fn()
                pump()

        stage_A0(0)
        stage_A0a(1)
        parts0 = A1_parts(0)
        win_pieces(2)
        run(parts0[0:2])
        win_pieces(3)
        run(parts0[2:4])
        win_pieces(0)
        win_pieces(1)
        run(parts0[4:8])
        win_pieces(4)
        run(parts0[8:10])
        A("sp", lambda e: e.dma_start(out=grow[:, 0:1024], in_=grow_d[:, 2048:3072]), w=["grow2"], dma="const2", total=True)
        tns_, keys_, dkey_ = slots["list"][cnt["st"] % len(slots["list"])]
        cnt["st"] += 1
        A("sp", lambda e: e.dma_start(out=tns_[:, :], in_=sgw_d[:, :]), w=keys_, dma=dkey_)
        A("dve", lambda e: e.tensor_tensor(out=wsg[:], in0=v3(tns_[:, :]), in1=causal.unsqueeze(1).to_broadcast([128, 8, 128]),
                                           op=ALU.mult), r=keys_ + ["causal"], w=["wsg"])
        slots["list"] = slots["list"][:NST]
        cnt["st"] = 0
        stage_A0b(1)
        load_x(2)
        load_x(3)
        stage_Bk(0)
        stage_Q1(0)
        stage_Q2(0)
        for i in range(NT):
            nun = 8 * ((i + 4) // 4)

            def P(fr, nun=nun):
                return int(fr * nun)
            parts = A1_parts(i + 1) if i + 1 < NT else [None] * 10
            groups = []
            if i == 0:
                for c in range(8):
                    groups.append([lambda c=c: load_cast(wout_d[:, c * 1024:(c + 1) * 1024], wout[:, c, :], 1024, f"wout{c}",
                                                         eng=("act", "dve")[c % 2], gain_cols=[8 + c])])
            groups.append([(lambda i=i: stage_A0a(i + 2)) if i + 2 < NT else None, parts[0]])
            for p_ in parts[1:7]:
                groups.append([p_])
            groups.append([parts[7],
                           (lambda i=i: stage_Da(i - 1)) if i >= 1 else None,
                           lambda i=i: stage_SGU(i),
                           parts[8]])
            groups.append([parts[9]])
            if i >= 1:
                def dfn(i=i):
                    stage_Db(i - 1)
                    if i + 3 < NT:
                        load_x(i + 3)
                groups.append([dfn])
            if i + 2 < NT:
                groups.append([lambda i=i: stage_A0b(i + 2)])
            if i == NT - 1:
                groups.append([lambda: ffn_prefetch(0, 0)])
                groups.append([lambda: ffn_f1_pre(0, 0)])
                for tt_ in range(8):
                    g_ = []
                    if tt_ + 1 < 8:
                        g_.append(lambda tt_=tt_: ffn_f1_pre(0, tt_ + 1))
                    g_.append(lambda tt_=tt_: ffn_f1_post(0, tt_))
                    groups.append(g_)
            groups = [[fn for fn in g_ if fn is not None] for g_ in groups]
            groups = [g_ for g_ in groups if g_]
            inserts = []
            for k_, g_ in enumerate(groups):
                for fn in g_:
                    inserts.append((P(0.04 + 0.71 * k_ / max(1, len(groups))), fn))
            if i + 1 < NT:
                p_ = max(P(0.76), inserts[-1][0])
                inserts.append((p_, lambda i=i: stage_Q1a(i + 1)))
                inserts.append((p_, lambda i=i: stage_Bk(i + 1)))
                inserts.append((max(P(0.86), p_ + 2), lambda i=i: stage_Q1b(i + 1)))
                inserts.append((nun - 1, lambda i=i: stage_Q2(i + 1)))
            stage_C(i, inserts)
            if debug and i == dbg_tile:
                s2 = i % 2
                for nm, src, keys in (("d_u", u_sbs[s2][:], [f"u{s2}"]), ("d_vln", vlns[s2][:], [f"vln{s2}"]),
                                      ("d_qaug", qaug[s2][:].rearrange("p h d -> p (h d)"), [f"qaug{s2}"]),
                                      ("d_kaug", kaug[s2][:].rearrange("p h d -> p (h d)"), [f"kaug{s2}", f"kaugoh{s2}"]),
                                      ("d_ycat", ycat[:], ["ycatA", "ycatB"]), ("d_ob", ob[:], ["ob0", "ob1"]),
                                      ("d_qT", qT[s2][:].rearrange("p h d -> p (h d)"), [f"qT{s2}", f"qTm{s2}"])):
                    A("sp", lambda e, nm=nm, src=src: e.dma_start(out=dbg[nm][:, :], in_=src), r=keys, dma="dbg", total=True)
        flush()

        def ffn_gu(hf, hook=None):
            def pe_act(f, ws, tg):
                def fg(e):
                    for c in range(8):
                        ins = e.matmul(SB[tg][:, :], lhsT=wgb[ws][:, c, :], rhs=h2T[:, c, tg * 512:(tg + 1) * 512],
                                       start=(c == 0), stop=(c == 7))
                    return ins
                A("pe", fg, r=[f"wgb{ws}"] + [f"h2T{4 * tg + q_}" for q_ in range(4)], w=[f"S{tg}"])

                def fu(e):
                    for c in range(8):
                        ins = e.matmul(O[tg][:, :], lhsT=wub[ws][:, c, :], rhs=h2T[:, c, tg * 512:(tg + 1) * 512],
                                       start=(c == 0), stop=(c == 7))
                    return ins
                A("pe", fu, r=[f"wub{ws}"] + [f"h2T{4 * tg + q_}" for q_ in range(4)], w=[f"O{tg}"] + (["O0misc"] if tg == 0 else []))
                A("act", lambda e: e.activation(out=sgs[tg][:], in_=SB[tg][:, :], func=AF.Silu), r=[f"S{tg}"], w=[f"sgs{tg}", ("qf", "kf")[tg]])

            def prod(f, tg):
                A("dve", lambda e: e.tensor_tensor(out=aT[:, f, tg * 512:(tg + 1) * 512], in0=O[tg][:, :], in1=sgs[tg][:],
                                                   op=ALU.mult), r=[f"O{tg}", f"sgs{tg}", "ffnbar"], w=[f"aT{f}_{tg}"])

            for f in range(NF):
                ws = f % 2
                if f == 0 and hook is not None:
                    pe_act(f, ws, 0)
                    pe_act(f, ws, 1)
                    hook()
                    ffn_prefetch(hf, 1)
                    prod(f, 0)
                    prod(f, 1)
                    continue
                if f + 1 < NF:
                    ffn_prefetch(hf, f + 1)
                for tg in range(2):
                    pe_act(f, ws, tg)
                    prod(f, tg)

        def ffn_down(hf):
            for tt in range(8):
                t = hf * 8 + tt
                xs = t % NXT
                A("sp", lambda e, t=t, xs=xs: e.dma_start(out=xt[xs][:], in_=x1_d[t * 128:(t + 1) * 128, :]),
                  r=[f"x1d{t}"], w=[f"xt{xs}"], dma=f"xt{xs}")
                if hf == 0:
                    ffn_f1_pre(1, tt)
                for dh in range(2):
                    gb = nextG()

                    def fd(e, gb=gb, dh=dh, tt=tt):
                        for f in range(NF):
                            ins = e.matmul(G[gb][:, :], lhsT=aT[:, f, tt * 128:(tt + 1) * 128], rhs=wdb[:, f, dh * 512:(dh + 1) * 512],
                                           start=(f == 0), stop=(f == NF - 1))
                        return ins
                    A("pe", fd, r=[f"aT{f}_{tt // 4}" for f in range(NF)] + [f"wdb{f}" for f in range(NF)], w=[f"G{gb}"])
                    A("dve", lambda e, gb=gb, dh=dh, xs=xs: e.tensor_tensor(out=ot[:, dh * 512:(dh + 1) * 512], in0=G[gb][:, :],
                                                                            in1=xt[xs][:, dh * 512:(dh + 1) * 512], op=ALU.add),
                      r=[f"G{gb}", f"xt{xs}", "ffnbar"], w=[f"x1t{dh}"])
                A("sp", lambda e, t=t: e.dma_start(out=out_d[t * 128:(t + 1) * 128, :], in_=ot[:]), r=["x1t0", "x1t1"], w=[f"outd{t}"], dma="ot")
                if hf == 0:
                    ffn_f1_post(1, tt)

        def end_of_loop():
            stage_D(NT - 1)
            flush()
            A("dve", lambda e: e.memset(st["ss2"][:, 1:2], 0.0), w=LOOPKEYS + ["ffnbar"], after_all=True)

        ffn_gu(0, hook=end_of_loop)
        ffn_prefetch(1, 0)
        ffn_down(0)
        ffn_gu(1)
        ffn_down(1)

        sems = {e: es.enter_context(nc.semaphore("s_" + e)) for e in Sched.ENGS}
        dsems = {k: es.enter_context(nc.semaphore("d_" + k)) for k in S.dkeys}
        S.assign(sems, dsems)
        finals = [S.dtotals["ot"]] + ([S.dtotals["dbg"]] if debug else [])
        with nc.Block() as block:
            @block.sync
            def _(e):
                S.run_engine("sp", e, final_waits=finals)

            @block.scalar
            def _(e):
                S.run_engine("act", e)

            @block.vector
            def _(e):
                S.run_engine("dve", e)

            @block.gpsimd
            def _(e):
                S.run_engine("pool", e)

            @block.tensor
            def _(e):
                S.run_engine("pe", e)
    return nc


_NC_CACHE = {}


def kernel(x, norm1_g, w_in, sgu_ln_g, sgu_ln_b, sgu_w, sgu_b, q_norm_g, k_norm_g,
           out_norm_a_g, out_norm_b_g, w_out, norm2_g, w_gate, w_up, w_down):
    f32 = np.float32
    x = np.asarray(x, f32)
    w_in_r = np.ascontiguousarray(np.asarray(w_in, f32)[0].reshape(8, 128, 2560).transpose(1, 0, 2)).reshape(128, 8 * 2560)
    w_out_r = np.ascontiguousarray(np.asarray(w_out, f32)[0].reshape(8, 128, 1024).transpose(1, 0, 2)).reshape(128, 8 * 1024)
    wg_r = np.ascontiguousarray(np.asarray(w_gate, f32)[0].reshape(8, 128, NF, 128).transpose(2, 1, 0, 3)).reshape(NF, 128, 1024)
    wu_r = np.ascontiguousarray(np.asarray(w_up, f32)[0].reshape(8, 128, NF, 128).transpose(2, 1, 0, 3)).reshape(NF, 128, 1024)
    wd_r = np.ascontiguousarray(np.asarray(w_down, f32)[0].reshape(NF, 128, 1024))
    sgu_wT = np.ascontiguousarray(np.asarray(sgu_w, f32)[0].transpose(2, 0, 1)).reshape(128, 1024)
    sgu_bT = np.ascontiguousarray(np.asarray(sgu_b, f32)[0].T)
    rowc = np.concatenate([np.asarray(sgu_ln_g, f32)[0], np.asarray(sgu_ln_b, f32)[0],
                           np.asarray(q_norm_g, f32)[0], np.asarray(k_norm_g, f32)[0]])
    rowc = np.ascontiguousarray(np.broadcast_to(rowc[None, :], (128, 1152)))
    grows = np.concatenate([np.asarray(norm1_g, f32)[0], np.asarray(out_norm_a_g, f32)[0], np.asarray(out_norm_b_g, f32)[0],
                            np.asarray(norm2_g, f32)[0]])
    grows = np.ascontiguousarray(np.broadcast_to(grows[None, :], (128, 3072)))
    gout = np.concatenate([np.asarray(out_norm_a_g, f32)[0], np.asarray(out_norm_b_g, f32)[0]])
    gcols = np.ascontiguousarray(np.concatenate([np.asarray(norm1_g, f32)[0].reshape(8, 128).T, gout.reshape(8, 128).T], axis=1))
    if "nc" not in _NC_CACHE:
        _NC_CACHE["nc"] = build_nc()
    nc = _NC_CACHE["nc"]
    shared = {"w_in_r": w_in_r, "w_out_r": w_out_r, "wg_r": wg_r, "wu_r": wu_r, "wd_r": wd_r, "sgu_wT": sgu_wT,
              "sgu_bT": sgu_bT, "rowc": rowc, "grows": grows, "gcols": gcols}
    in_maps = [dict(shared, x=np.ascontiguousarray(x[b])) for b in range(8)]
    res = run_bass_kernel_spmd(nc, in_maps, core_ids=list(range(8)))
    return np.stack([np.asarray(r["out"], f32) for r in res.results], axis=0)
```

```python
import numpy as np
from contextlib import ExitStack

import concourse.bass as bass
import concourse.mybir as mybir
from concourse.bass_utils import run_bass_kernel_spmd

F32 = mybir.dt.float32
BF16 = mybir.dt.bfloat16
AF = mybir.ActivationFunctionType
ALU = mybir.AluOpType
AX = mybir.AxisListType

S_LEN = 2048
D = 1024
NT = 16
H = 8
HD = 64
DFF = 2816
NF = 22
EPS = 1e-6
NEG = -30000.0


class _Op:
    __slots__ = ("idx", "eng", "fn", "deps", "is_dma", "dkey", "signal", "ticket", "total")

    def __init__(self, idx, eng, fn, is_dma, dkey, total):
        self.idx = idx
        self.eng = eng
        self.fn = fn
        self.deps = []
        self.is_dma = is_dma
        self.dkey = dkey
        self.signal = False
        self.ticket = None
        self.total = total


class Sched:
    ENGS = ("pe", "act", "dve", "pool", "sp")

    def __init__(self):
        self.ops = []
        self.last_writer = {}
        self.readers = {}
        self.dkeys = []

    def add(self, eng, fn, r=(), w=(), dma=None, total=False, after_all=False):
        op = _Op(len(self.ops), eng, fn, dma is not None, dma, total)
        if dma is not None and dma not in self.dkeys:
            self.dkeys.append(dma)
        deps = {}
        if after_all:
            last = {}
            for o in self.ops:
                if not o.is_dma:
                    last[o.eng] = o
            for o in last.values():
                if o.eng != eng:
                    deps[o.idx] = o
        for k in r:
            x = self.last_writer.get(k)
            if x is not None:
                deps[x.idx] = x
        for k in w:
            x = self.last_writer.get(k)
            if x is not None:
                deps[x.idx] = x
            for rd in self.readers.get(k, ()):
                deps[rd.idx] = rd
        for k in r:
            self.readers.setdefault(k, []).append(op)
        for k in w:
            self.last_writer[k] = op
            self.readers[k] = []
        for d in deps.values():
            if d.eng == "pe" and eng == "pe" and not d.is_dma and dma is None:
                continue
            op.deps.append(d)
            d.signal = True
        self.ops.append(op)
        return op

    def assign(self, sems, dma_sems):
        cnt = {e: 0 for e in self.ENGS}
        dcnt = {}
        for op in self.ops:
            if op.is_dma:
                dcnt[op.dkey] = dcnt.get(op.dkey, 0) + 16
                op.ticket = (dma_sems[op.dkey], dcnt[op.dkey])
            elif op.signal:
                cnt[op.eng] += 1
                op.ticket = (sems[op.eng], cnt[op.eng])
        for op in self.ops:
            if op.is_dma and op.total:
                op.ticket = (dma_sems[op.dkey], dcnt[op.dkey])
        self.dtotals = {k: (dma_sems[k], v) for k, v in dcnt.items()}

    def run_engine(self, name, eng, final_waits=()):
        waited = {}
        for op in self.ops:
            if op.eng != name:
                continue
            for d in op.deps:
                sem, val = d.ticket
                if waited.get(id(sem), 0) >= val:
                    continue
                eng.wait_ge(sem, val)
                waited[id(sem)] = val
            ins = op.fn(eng)
            if op.is_dma:
                ins.then_inc(op.ticket[0], 16)
            elif op.signal:
                ins.then_inc(op.ticket[0], 1)
        for sem, val in final_waits:
            eng.wait_ge(sem, val)


def build_nc(debug=False, dbg_tile=0):
    nc = bass.Bass("TRN2", target_bir_lowering=False)
    dram = nc.dram_tensor
    x_d = dram("x", [S_LEN, D], F32, kind="ExternalInput").ap()
    win_d = dram("w_in_r", [128, 8 * 2560], F32, kind="ExternalInput").ap()
    wout_d = dram("w_out_r", [128, 8 * 1024], F32, kind="ExternalInput").ap()
    wg_d = dram("wg_r", [NF, 128, 1024], F32, kind="ExternalInput").ap()
    wu_d = dram("wu_r", [NF, 128, 1024], F32, kind="ExternalInput").ap()
    wd_d = dram("wd_r", [NF, 128, 1024], F32, kind="ExternalInput").ap()
    sgw_d = dram("sgu_wT", [128, 1024], F32, kind="ExternalInput").ap()
    sgb_d = dram("sgu_bT", [128, 8], F32, kind="ExternalInput").ap()
    rowc_d = dram("rowc", [128, 1152], F32, kind="ExternalInput").ap()
    grow_d = dram("grows", [128, 3072], F32, kind="ExternalInput").ap()
    gcol_d = dram("gcols", [128, 16], F32, kind="ExternalInput").ap()
    out_d = dram("out", [S_LEN, D], F32, kind="ExternalOutput").ap()
    x1_d = dram("x1s", [S_LEN, D], F32, kind="ExternalOutput" if debug else "Internal").ap()
    dbg = {}
    if debug:
        for nm, shp, dt_ in (("d_u", [128, 512], F32), ("d_vln", [128, 512], BF16), ("d_qaug", [128, 512], BF16),
                             ("d_kaug", [128, 576], BF16), ("d_ycat", [128, 1024], BF16), ("d_hb", [128, 1024], BF16),
                             ("d_ob", [128, 512], F32), ("d_qT", [128, 1024], BF16), ("d_kT", [128, 1024], BF16)):
            dbg[nm] = dram(nm, shp, dt_, kind="ExternalOutput").ap()

    S = Sched()
    A = S.add

    with ExitStack() as es:
        def sb(name, shape, dt=F32):
            return es.enter_context(nc.sbuf_tensor(name, shape, dt))

        def ps(name, shape, dt=F32):
            return es.enter_context(nc.psum_tensor(name, shape, dt))

        arena = sb("arena", [128, 53376], BF16)
        ar = arena[:]
        win = ar[:, 0:20480].rearrange("p (c n) -> p c n", c=8)
        wout = ar[:, 20480:28672].rearrange("p (c n) -> p c n", c=8)
        kT = ar[:, 28672:45056].rearrange("p (h t) -> p h t", h=8)
        Vv = ar[:, 45056:53376].rearrange("p (t h d) -> p t h d", t=16, h=8)
        h2T = ar[:, 0:8192].rearrange("p (c t) -> p c t", c=8)
        aT = ar[:, 8192:30720].rearrange("p (f t) -> p f t", f=NF)
        wdb = ar[:, 30720:53248].rearrange("p (f n) -> p f n", f=NF)

        NST = 3
        stage = [sb(f"stage{k}", [128, 1024]) for k in range(NST)]
        NXT = 4
        xt = [sb(f"xt{k}", [128, 1024]) for k in range(NXT)]
        hb = sb("hb", [128, 1024], BF16)
        hT = [sb(f"hT{k}", [128, 8, 128], BF16) for k in range(2)]
        u_sbs = [sb(f"u_sb{k}", [128, 512]) for k in range(2)]
        vg = sb("vg", [128, 512])
        sq = sb("sq", [128, 512])
        sqq = sb("sqq", [128, 512])
        sqk = sb("sqk", [128, 512])
        vlns = [sb(f"vln{k}", [128, 512], BF16) for k in range(2)]
        qf = sb("qf", [128, 512])
        kf = sb("kf", [128, 512])
        qaug = [sb(f"qaug{k}", [128, 8, 64], BF16) for k in range(2)]
        qmask = [sb(f"qmask{k}", [128, 8, 8], BF16) for k in range(2)]
        kaug = [sb(f"kaug{k}", [128, 8, 72], BF16) for k in range(2)]
        qT = [sb(f"qT{k}", [128, 8, 128], BF16) for k in range(2)]
        NPT = 3
        PT = [sb(f"PT{k}", [128, 512], BF16) for k in range(NPT)]
        ob = sb("ob", [128, 512])
        ya = sb("ya", [128, 512])
        identf = ya[:, 0:128]
        trif = ya[:, 128:256]
        causal = ya[:, 256:384]
        ycat = sb("ycat", [128, 1024], BF16)
        yT = sb("yT", [128, 8, 128], BF16)
        x1t = sb("x1t", [128, 1024])
        grow = sb("grow", [128, 1024])
        gcols = sb("gcols_sb", [128, 16])
        rowc = sb("rowc_sb", [128, 1152])
        sgb = sb("sgb", [128, 8])
        wsg = sb("wsg", [128, 8, 128], BF16)
        ident = sb("ident", [128, 128], BF16)
        trimask = sb("trimask", [128, 128], BF16)
        mhalf = sb("mhalf", [128, 8])
        onescol = sb("onescol", [128, 1], BF16)
        gate_sb = sb("gate_sb", [128, 8, 8])
        top8 = sb("top8", [128, 8, 8])
        sel = sb("sel", [128, 8, 8])
        kmT = sb("kmT", [64, 8, 8], BF16)
        kmpart = sb("kmpart", [64, 8])
        st = {n: sb("st_" + n, [128, 8]) for n in
              ("ss1", "rs1", "vsum", "vneg", "vss", "vrs", "qss", "qrs", "kss", "krs",
               "ass", "ars", "bss", "brs", "osum", "ss2", "rs2")}
        wgb = [hT[0], hT[1]]
        wub = [qT[0], qT[1]]
        sgs = [qf, kf]
        ot = x1t

        G = [ps(f"G{k}", [128, 512]) for k in range(3)]
        SB = [ps(f"S{k}", [128, 512]) for k in range(3)]

        def TB(gb):
            return G[gb][:, :].bitcast(BF16)
        O = [ps(f"O{k}", [128, 512]) for k in range(2)]

        g2row = grow[:, 0:1024]
        lng = rowc[:, 0:512]
        lnb = rowc[:, 512:1024]
        qg = rowc[:, 1024:1088]
        kg = rowc[:, 1088:1152]

        cnt = {"g": 0, "t": 0, "st": 0, "cast": 0}

        open_g = set()

        def nextG():
            for _ in range(len(G)):
                cnt["g"] += 1
                gb = (cnt["g"] - 1) % len(G)
                if gb not in open_g:
                    return gb
            raise RuntimeError("all general PSUM banks hold open accumulation groups")

        def nextT():
            cnt["t"] += 1
            return (cnt["t"] - 1) % 2

        def v3(ap, h=8):
            return ap.rearrange("p (h d) -> p h d", h=h)

        def bc(ap, n):
            return ap.unsqueeze(2).to_broadcast([ap.shape[0], ap.shape[1], n])

        slots = {"list": [(stage[k], [f"stage{k}"], f"stage{k}") for k in range(NST)] + [(x1t, ["x1t0", "x1t1"], "x1t"), (xt[3], ["xt3"], "xt3")]}

        def load_cast(src_ap, dst_ap, width, dst_key, eng=None, rkeys=(), gain_cols=None):
            rkeys = list(rkeys)
            dkeys_ = list(dst_key) if isinstance(dst_key, (list, tuple)) else [dst_key]
            sl_list = slots["list"]
            tns, keys, dkey = sl_list[cnt["st"] % len(sl_list)]
            cnt["st"] += 1
            dst_stage = tns[:, 0:width]
            if len(src_ap.shape) == 3:
                dst_stage = dst_stage.rearrange("p (c n) -> p c n", c=src_ap.shape[1])
            A("sp", lambda e: e.dma_start(out=dst_stage, in_=src_ap), w=keys, dma=dkey)
            if eng is None:
                eng = "pool"
            src = tns[:, 0:width]
            if len(dst_ap.shape) == 3:
                src = src.rearrange("p (c n) -> p c n", c=dst_ap.shape[1])
            if gain_cols is not None:
                nchunk = len(gain_cols)
                wch = width // nchunk
                for j, gc in enumerate(gain_cols):
                    d_ap = dst_ap[:, j, :] if len(dst_ap.shape) == 3 else dst_ap
                    s_ap = tns[:, j * wch:(j + 1) * wch]
                    gap = gcols[:, gc:gc + 1]
                    if eng == "act":
                        A("act", lambda e, d_ap=d_ap, s_ap=s_ap, gap=gap: e.activation(out=d_ap, in_=s_ap, func=AF.Copy, scale=gap),
                          r=keys + rkeys + ["gcols"], w=[f"{dkeys_[0]}#{j}"])
                    else:
                        A(eng, lambda e, d_ap=d_ap, s_ap=s_ap, gap=gap: e.tensor_scalar(out=d_ap, in0=s_ap, scalar1=gap, scalar2=None,
                                                                                         op0=ALU.mult),
                          r=keys + rkeys + ["gcols"], w=[f"{dkeys_[0]}#{j}"])
            elif eng == "act":
                A("act", lambda e: e.activation(out=dst_ap, in_=src, func=AF.Copy), r=keys + rkeys, w=dkeys_)
            else:
                A(eng, lambda e: e.tensor_copy(out=dst_ap, in_=src), r=keys + rkeys, w=dkeys_)

        def rstd_chain(ss, rs, n, cols=8):
            A("pool", lambda e: e.tensor_scalar(out=st[rs][:, 0:cols], in0=st[ss][:, 0:cols], scalar1=1.0 / n, scalar2=EPS,
                                                op0=ALU.mult, op1=ALU.add), r=[ss], w=[rs])
            A("pool", lambda e: e.tensor_tensor(out=st[rs][:, 0:cols], in0=st[rs][:, 0:cols], in1=mhalf[:, 0:cols], op=ALU.pow),
              r=[rs, "mhalf"], w=[rs])

        def rstd_steps(ss, rs, n, cols=8):
            return [
                lambda: A("pool", lambda e: e.tensor_scalar(out=st[rs][:, 0:cols], in0=st[ss][:, 0:cols], scalar1=1.0 / n, scalar2=EPS,
                                                            op0=ALU.mult, op1=ALU.add), r=[ss], w=[rs]),
                lambda: A("pool", lambda e: e.tensor_tensor(out=st[rs][:, 0:cols], in0=st[rs][:, 0:cols], in1=mhalf[:, 0:cols], op=ALU.pow),
                          r=[rs, "mhalf"], w=[rs]),
            ]

        chains = []

        def defer(name, steps):
            chains.append({"name": name, "steps": list(steps)})

        def pump():
            for ch in list(chains):
                if ch["steps"]:
                    ch["steps"].pop(0)()
                if not ch["steps"]:
                    chains.remove(ch)

        def flush(name=None):
            for ch in list(chains):
                if name is None or ch["name"] == name:
                    while ch["steps"]:
                        ch["steps"].pop(0)()
                    chains.remove(ch)

        A("pool", lambda e: e.memset(identf, 1.0), w=["identf"])
        A("pool", lambda e: e.affine_select(out=identf, in_=identf, pattern=[[-1, 128]], compare_op=ALU.is_equal,
                                            fill=0.0, base=0, channel_multiplier=1), r=["identf"], w=["identf"])
        A("pool", lambda e: e.tensor_copy(out=ident[:], in_=identf), r=["identf"], w=["ident"])
        A("pool", lambda e: e.memset(causal, 1.0), w=["causal"])
        A("pool", lambda e: e.affine_select(out=causal, in_=causal, pattern=[[1, 128]], compare_op=ALU.is_ge,
                                            fill=0.0, base=0, channel_multiplier=-1), r=["causal"], w=["causal"])
        A("pool", lambda e: e.memset(trif, 0.0), w=["trif"])
        A("pool", lambda e: e.affine_select(out=trif, in_=trif, pattern=[[1, 128]], compare_op=ALU.is_ge,
                                            fill=NEG, base=0, channel_multiplier=-1), r=["trif"], w=["trif"])
        A("pool", lambda e: e.tensor_copy(out=trimask[:], in_=trif), r=["trif"], w=["trimask"])
        A("pool", lambda e: e.memset(mhalf[:], -0.5), w=["mhalf"])
        A("pool", lambda e: e.memset(onescol[:], 1.0 / 256.0), w=["onescol"])
        A("pool", lambda e: e.memset(gate_sb[:], -1e30), w=["gate_sb"])
        A("pool", lambda e: e.memset(kmT[:], 0.0), w=["kmT"])
        for k in range(2):
            A("pool", lambda e, k=k: e.memset(qmask[k][:], 0.0), w=[f"qmask{k}"])
            A("pool", lambda e, k=k: e.memset(qT[k][64:72, :, :], 0.0), w=[f"qTm{k}"])
        A("pool", lambda e: e.memset(Vv[:, :, :, 64:65], 1.0), w=["Vones"])

        A("sp", lambda e: e.dma_start(out=xt[0][:], in_=x_d[0:128, :]), w=["xt0"], dma="xt0")
        A("sp", lambda e: e.dma_start(out=gcols[:], in_=gcol_d[:, :]), w=["gcols"], dma="const", total=True)
        A("sp", lambda e: e.dma_start(out=xt[1][:], in_=x_d[128:256, :]), w=["xt1"], dma="xt1")
        A("sp", lambda e: e.dma_start(out=rowc[:], in_=rowc_d[:, :]), w=["rowc"], dma="const", total=True)
        A("sp", lambda e: e.dma_start(out=sgb[:], in_=sgb_d[:, :]), w=["sgb"], dma="const", total=True)
        A("pool", lambda e: e.tensor_tensor(out=kg, in0=kg, in1=qg, op=ALU.mult), r=["rowc"], w=["rowc"])

        def load_x(i):
            k = i % NXT
            A("sp", lambda e: e.dma_start(out=xt[k][:], in_=x_d[i * 128:(i + 1) * 128, :]), w=[f"xt{k}"], dma=f"xt{k}")

        WOUTKEYS = [f"wout{c}#0" for c in range(8)]
        win_d3 = win_d.rearrange("p (c n) -> p c n", c=8)
        pcnt = {"n": 0}

        def win_pieces(sl):
            for cp in range(4):
                key = f"win{sl}_{cp}"
                load_cast(win_d3[:, 2 * cp:2 * cp + 2, sl * 512:(sl + 1) * 512], win[:, 2 * cp:2 * cp + 2, sl * 512:(sl + 1) * 512],
                          1024, key, eng=("dve", "act")[pcnt["n"] % 2], gain_cols=[2 * cp, 2 * cp + 1])
                pcnt["n"] += 1
                pump()


        def stage_A0a(i, deferred=True):
            k = i % NXT
            xk = f"xt{k}"
            steps = [lambda: A("act", lambda e: e.activation(out=hb[:], in_=xt[k][:], func=AF.Square, accum_out=st["ss1"][:, 0:1]),
                               r=[xk], w=["hb", "ss1"])]
            steps += rstd_steps("ss1", "rs1", 1024.0, cols=1)
            steps.append(lambda: A("dve", lambda e: e.tensor_scalar(out=hb[:], in0=xt[k][:], scalar1=st["rs1"][:, 0:1], scalar2=None,
                                                                    op0=ALU.mult), r=[xk, "rs1"], w=["hb"]))
            defer("a0", steps)
            if not deferred:
                flush("a0")

        def stage_A0b(i):
            s2 = i % 2
            flush("a0")

            tb = nextG()

            def tr(e):
                for c in range(8):
                    ins = e.transpose(out=TB(tb)[:, c * 128:(c + 1) * 128], in_=hb[:, c * 128:(c + 1) * 128], identity=ident[:])
                return ins
            A("pe", tr, r=["hb", "ident"], w=[f"G{tb}"])
            A("dve", lambda e: e.tensor_copy(out=hT[s2][:].rearrange("p c t -> p (c t)"), in_=TB(tb)[:, :]), r=[f"G{tb}"], w=[f"hT{s2}"])

        def stage_A0(i):
            stage_A0a(i, deferred=False)
            stage_A0b(i)

        def A1_parts(i):
            s2 = i % 2
            parts = []

            def proj_half(sl, gb, half):
                def f(e):
                    for c in range(4 * half, 4 * half + 4):
                        ins = e.matmul(G[gb][:, :], lhsT=hT[s2][:, c, :], rhs=win[:, c, sl * 512:(sl + 1) * 512],
                                       start=(c == 0), stop=(c == 7))
                    return ins
                return lambda: A("pe", f, r=[f"hT{s2}"] + [f"win{sl}_{2 * half + a_}#{b_}" for a_ in range(2) for b_ in range(2)],
                                 w=[f"G{gb}"])

            def qk_slice(nm, sl, src, sqb, ssn, rsn, gvec, dst, dkey):
                gbox = {}

                def first():
                    gbox["gb"] = nextG()
                    open_g.add(gbox["gb"])
                    proj_half(sl, gbox["gb"], 0)()

                def second():
                    gb = gbox["gb"]
                    proj_half(sl, gb, 1)()
                    open_g.discard(gb)
                    A("dve", lambda e: e.tensor_copy(out=src[:], in_=G[gb][:, :]), r=[f"G{gb}"], w=[nm + "f"])
                    steps = [
                        lambda: A("pool", lambda e: e.tensor_tensor(out=sqb[:], in0=src[:], in1=src[:], op=ALU.mult), r=[nm + "f"], w=["sq" + nm]),
                        lambda: A("dve", lambda e: e.tensor_reduce(out=st[ssn][:], in_=v3(sqb[:]), axis=AX.X, op=ALU.add), r=["sq" + nm], w=[ssn]),
                    ] + rstd_steps(ssn, rsn, 64.0)
                    if gvec is None:
                        steps.append(lambda: A("dve", lambda e: e.tensor_tensor(out=dst, in0=v3(src[:]), in1=bc(st[rsn][:], 64), op=ALU.mult),
                                               r=[nm + "f", rsn], w=[dkey]))
                    else:
                        steps.append(lambda: A("dve", lambda e: e.tensor_tensor(out=v3(src[:]), in0=v3(src[:]), in1=bc(st[rsn][:], 64),
                                                                                op=ALU.mult), r=[nm + "f", rsn], w=[nm + "f"]))
                        steps.append(lambda: A("dve", lambda e: e.tensor_tensor(out=dst, in0=v3(src[:]),
                                                                                in1=gvec.unsqueeze(1).to_broadcast([128, 8, 64]),
                                                                                op=ALU.mult), r=[nm + "f", "rowc"], w=[dkey]))
                    defer(nm + "chain", steps)
                return [first, second]

            def gen_slice(sl, evac):
                gbox = {}

                def first():
                    gbox["gb"] = nextG()
                    open_g.add(gbox["gb"])
                    proj_half(sl, gbox["gb"], 0)()

                def second_pe():
                    proj_half(sl, gbox["gb"], 1)()
                    open_g.discard(gbox["gb"])

                def second_evac():
                    evac(gbox["gb"])

                def second():
                    second_pe()
                    second_evac()
                return [first, second, second_pe, second_evac]

            def evac_u(gb):
                A("act", lambda e: e.activation(out=u_sbs[s2][:], in_=G[gb][:, :], func=AF.Gelu_apprx_tanh), r=[f"G{gb}"], w=[f"u{s2}"])

            def evac_va(gb):
                A("act", lambda e: e.activation(out=vg[:], in_=G[gb][:, :], func=AF.Gelu_apprx_tanh), r=[f"G{gb}"], w=["vg"])
                flush("qchain")
                steps = [
                    lambda: A("dve", lambda e: e.tensor_reduce(out=st["vsum"][:], in_=v3(vg[:]), axis=AX.X, op=ALU.add), r=["vg"], w=["vsum"]),
                    lambda: A("pool", lambda e: e.tensor_scalar(out=st["vneg"][:], in0=st["vsum"][:], scalar1=-1.0 / 64, scalar2=None,
                                                                op0=ALU.mult), r=["vsum"], w=["vneg"]),
                    lambda: A("dve", lambda e: e.tensor_tensor(out=v3(vg[:]), in0=v3(vg[:]), in1=bc(st["vneg"][:], 64), op=ALU.add),
                              r=["vg", "vneg"], w=["vg"]),
                    lambda: A("pool", lambda e: e.tensor_tensor(out=sqq[:], in0=vg[:], in1=vg[:], op=ALU.mult), r=["vg"], w=["sqq"]),
                    lambda: A("dve", lambda e: e.tensor_reduce(out=st["vss"][:], in_=v3(sqq[:]), axis=AX.X, op=ALU.add), r=["sqq"], w=["vss"]),
                ] + rstd_steps("vss", "vrs", 64.0) + [
                    lambda: A("dve", lambda e: e.tensor_tensor(out=v3(vg[:]), in0=v3(vg[:]), in1=bc(st["vrs"][:], 64), op=ALU.mult),
                              r=["vg", "vrs"], w=["vg"]),
                    lambda: A("pool", lambda e: e.tensor_tensor(out=vg[:], in0=vg[:], in1=lng, op=ALU.mult), r=["vg", "rowc"], w=["vg"]),
                    lambda: A("dve", lambda e: e.tensor_tensor(out=vlns[s2][:], in0=vg[:], in1=lnb, op=ALU.add), r=["vg", "rowc"], w=[f"vln{s2}"]),
                ]
                defer(f"ln{s2}", steps)

            def evac_vb(gb):
                A("dve", lambda e: e.tensor_copy(out=Vv[:, i, :, 0:64], in_=v3(G[gb][:, :])), r=[f"G{gb}"], w=[f"V{i}"])

            pq = qk_slice("q", 2, qf, sqq, "qss", "qrs", None, qaug[s2][:, :, :], f"qaug{s2}")
            pk = qk_slice("k", 3, kf, sqk, "kss", "krs", kg, kaug[s2][:, :, 0:64], f"kaug{s2}")
            nblk = i // 2

            def k_first():
                A("pool", lambda e: e.memset(kaug[s2][:, :, 64:72], 0.0), w=[f"kaugoh{s2}"])
                A("pool", lambda e: e.memset(kaug[s2][:, :, 64 + nblk:65 + nblk], 1.0), w=[f"kaugoh{s2}"])
                pk[0]()
            pu = gen_slice(0, evac_u)
            pva = gen_slice(1, evac_va)
            pvb = gen_slice(4, evac_vb)

            def uva_pe():
                pu[2]()
                pva[2]()

            def uva_evac():
                pu[3]()
                pva[3]()
            parts = [pq[0], pq[1], k_first, pk[1], pu[0], pva[0], uva_pe, uva_evac, pvb[0], pvb[1]]
            return parts

        def stage_Bk(i):
            s2 = i % 2
            n = i // 2
            flush("kchain")

            tb = nextG()

            def trk(e):
                for h in range(8):
                    ins = e.transpose(out=TB(tb)[0:72, h * 128:(h + 1) * 128], in_=kaug[s2][:, h, :], identity=ident[:])
                return ins
            A("pe", trk, r=[f"kaug{s2}", f"kaugoh{s2}", "ident"], w=[f"G{tb}"])
            A("dve", lambda e: e.tensor_copy(out=kT[0:72, :, i * 128:(i + 1) * 128], in_=v3(TB(tb)[0:72, :])), r=[f"G{tb}"], w=[f"kT{i}"])

            def km(e):
                for h in range(8):
                    ins = e.matmul(O[0][0:64, 300 + h:301 + h], lhsT=kaug[s2][:, h, 0:64], rhs=onescol[:, :], start=True, stop=True)
                return ins
            A("pe", km, r=[f"kaug{s2}", "onescol"], w=["O0misc"])
            if i % 2 == 0:
                A("dve", lambda e: e.tensor_copy(out=kmpart[:], in_=O[0][0:64, 300:308]), r=["O0misc"], w=["kmpart"])
            else:
                A("dve", lambda e: e.tensor_tensor(out=kmT[:, :, n], in0=O[0][0:64, 300:308], in1=kmpart[:], op=ALU.add),
                  r=["O0misc", "kmpart"], w=["kmT"])

        def stage_Q1a(i):
            s2 = i % 2
            flush("qchain")
            tb = nextG()

            def trq(e):
                for h in range(8):
                    ins = e.transpose(out=TB(tb)[0:64, h * 128:(h + 1) * 128], in_=qaug[s2][:, h, :], identity=ident[:])
                return ins
            A("pe", trq, r=[f"qaug{s2}", "ident"], w=[f"G{tb}"])
            A("dve", lambda e: e.tensor_copy(out=qT[s2][0:64, :, :], in_=v3(TB(tb)[0:64, :])), r=[f"G{tb}"], w=[f"qT{s2}"])

        def stage_Q1b(i):
            s2 = i % 2
            qb = i // 2
            if i >= 8:
                def gm(e):
                    for h in range(8):
                        ins = e.matmul(O[0][:, 320 + h * 8:320 + h * 8 + qb], lhsT=qT[s2][0:64, h, :], rhs=kmT[:, h, 0:qb],
                                       start=True, stop=True)
                    return ins
                A("pe", gm, r=[f"qT{s2}", "kmT"], w=["O0misc"])
                A("dve", lambda e: e.tensor_copy(out=gate_sb[:, :, 0:qb], in_=v3(O[0][:, 320:384])[:, :, 0:qb]), r=["O0misc"], w=["gate_sb"])
                for h in range(8):
                    A("dve", lambda e, h=h: e.max(out=top8[:, h, :], in_=gate_sb[:, h, :]), r=["gate_sb"], w=[f"top8_{h}"])
                A("dve", lambda e: e.tensor_tensor(out=sel[:, :, 0:qb], in0=gate_sb[:, :, 0:qb],
                                                   in1=top8[:, :, 2:3].to_broadcast([128, 8, qb]), op=ALU.is_ge),
                  r=["gate_sb"] + [f"top8_{h}" for h in range(8)], w=["sel"])
                A("dve", lambda e: e.tensor_scalar(out=qmask[s2][:, :, 0:qb], in0=sel[:, :, 0:qb], scalar1=1.0, scalar2=-NEG,
                                                   op0=ALU.subtract, op1=ALU.mult), r=["sel"], w=[f"qmask{s2}"])

        def stage_Q1(i):
            stage_Q1a(i)
            stage_Q1b(i)

        def stage_Q2(i):
            s2 = i % 2
            if i >= 8:
                tb = nextG()

                def trm(e):
                    for h in range(8):
                        ins = e.transpose(out=TB(tb)[64:72, h * 128:(h + 1) * 128], in_=qmask[s2][:, h, :], identity=ident[:])
                    return ins
                A("pe", trm, r=[f"qmask{s2}", "ident"], w=[f"G{tb}"])
                A("dve", lambda e: e.tensor_copy(out=qT[s2][64:72, :, :], in_=v3(TB(tb)[64:72, :])), r=[f"G{tb}"], w=[f"qTm{s2}"])

        def stage_SGU(i):
            s2 = i % 2
            flush(f"ln{s2}")
            gb = nextG()

            def mix(e):
                for h in range(8):
                    ins = e.matmul(G[gb][:, h * 64:(h + 1) * 64], lhsT=wsg[:, h, :], rhs=vlns[s2][:, h * 64:(h + 1) * 64],
                                   start=True, stop=True)
                return ins
            A("pe", mix, r=["wsg", f"vln{s2}"], w=[f"G{gb}"])
            A("dve", lambda e: e.tensor_tensor(out=v3(ya[:]), in0=v3(G[gb][:, :]), in1=bc(sgb[:], 64), op=ALU.add),
              r=[f"G{gb}", "sgb"], w=["ya", "identf", "trif", "causal"])
            flush("kchain")
            steps = [
                lambda: A("dve", lambda e: e.tensor_tensor(out=ya[:], in0=ya[:], in1=u_sbs[s2][:], op=ALU.mult), r=["ya", f"u{s2}"], w=["ya"]),
                lambda: A("pool", lambda e: e.tensor_tensor(out=sqk[:], in0=ya[:], in1=ya[:], op=ALU.mult), r=["ya"], w=["sqk"]),
                lambda: A("dve", lambda e: e.tensor_reduce(out=st["ass"][:], in_=v3(sqk[:]), axis=AX.X, op=ALU.add), r=["sqk"], w=["ass"]),
            ] + rstd_steps("ass", "ars", 64.0) + [
                lambda: A("dve", lambda e: e.tensor_tensor(out=v3(ycat[:, 0:512]), in0=v3(ya[:]), in1=bc(st["ars"][:], 64), op=ALU.mult),
                          r=["ya", "ars"], w=["ycatA"]),
            ]
            defer("sgu", steps)

        def stage_C(i, inserts):
            s2 = i % 2
            ng = (i + 4) // 4
            units = [(h, g) for h in range(8) for g in range(ng)]
            NS = len(SB)

            def emit_qk(u):
                h, g = units[u]
                sbk = u % NS
                js = list(range(4 * g, min(4 * g + 4, i + 1)))

                def f(e):
                    for jj, j in enumerate(js):
                        diag = (j == i)
                        ins = e.matmul(SB[sbk][:, jj * 128:(jj + 1) * 128], lhsT=kT[0:72, h, j * 128:(j + 1) * 128],
                                       rhs=qT[s2][0:72, h, :], start=True, stop=not diag)
                        if diag:
                            ins = e.matmul(SB[sbk][:, jj * 128:(jj + 1) * 128], lhsT=ident[:, :], rhs=trimask[:, :],
                                           start=False, stop=True)
                    return ins
                A("pe", f, r=[f"kT{j}" for j in js] + [f"qT{s2}", f"qTm{s2}", "ident", "trimask"], w=[f"S{sbk}"])

            def emit_exp_pv(u):
                h, g = units[u]
                sbk = u % NS
                pk = u % NPT
                js = list(range(4 * g, min(4 * g + 4, i + 1)))
                wdt = len(js) * 128
                A("act", lambda e: e.activation(out=PT[pk][:, 0:wdt], in_=SB[sbk][:, 0:wdt], func=AF.Exp, scale=0.125),
                  r=[f"S{sbk}"], w=[f"PT{pk}"])
                ohalf = h // 4
                hh = h % 4

                def f(e):
                    for jj, j in enumerate(js):
                        ins = e.matmul(O[ohalf][:, hh * 65:(hh + 1) * 65], lhsT=PT[pk][:, jj * 128:(jj + 1) * 128],
                                       rhs=Vv[:, j, h, :], start=(j == 0), stop=(j == i))
                    return ins
                wk = [f"O{ohalf}"] + (["O0misc"] if ohalf == 0 else [])
                A("pe", f, r=[f"PT{pk}", "Vones"] + [f"V{j}" for j in js], w=wk)

            def emit_oevac(ohalf):
                ov = O[ohalf][:, 0:260].rearrange("p (h d) -> p h d", h=4)
                A("dve", lambda e: e.reciprocal(out=st["osum"][:, ohalf * 4:(ohalf + 1) * 4], in_=ov[:, :, 64]),
                  r=[f"O{ohalf}"], w=[f"osum{ohalf}"])
                A("dve", lambda e: e.tensor_tensor(out=v3(ob[:, ohalf * 256:(ohalf + 1) * 256], h=4), in0=ov[:, :, 0:64],
                                                   in1=bc(st["osum"][:, ohalf * 4:(ohalf + 1) * 4], 64), op=ALU.mult),
                  r=[f"O{ohalf}", f"osum{ohalf}"], w=[f"ob{ohalf}"])

            ins_by_pos = {}
            for pos, fn in inserts:
                ins_by_pos.setdefault(min(pos, len(units) - 1), []).append(fn)
            AHEAD = NS - 1
            for u in range(min(AHEAD, len(units))):
                emit_qk(u)
            for u in range(len(units)):
                if u + AHEAD < len(units):
                    emit_qk(u + AHEAD)
                emit_exp_pv(u)
                h, g = units[u]
                if g == ng - 1 and h == 3:
                    flush("tail")
                    emit_oevac(0)
                if g == ng - 1 and h == 7:
                    emit_oevac(1)
                for fn in ins_by_pos.get(u, ()):
                    fn()
                if len(units) < 24 or u % 3 != 2:
                    pump()
            flush("ln0")
            flush("ln1")
            flush("sgu")
            steps = [
                lambda: A("pool", lambda e: e.tensor_tensor(out=sq[:], in0=ob[:], in1=ob[:], op=ALU.mult), r=["ob0", "ob1"], w=["sq"]),
                lambda: A("dve", lambda e: e.tensor_reduce(out=st["bss"][:], in_=v3(sq[:]), axis=AX.X, op=ALU.add), r=["sq"], w=["bss"]),
            ] + rstd_steps("bss", "brs", 64.0) + [
                lambda: A("dve", lambda e: e.tensor_tensor(out=v3(ycat[:, 512:1024]), in0=v3(ob[:]), in1=bc(st["brs"][:], 64), op=ALU.mult),
                          r=["ob0", "ob1", "brs"], w=["ycatB"]),
            ]
            defer("tail", steps)

        def stage_Da(i):
            flush("tail")
            flush("sgu")
            tb = nextG()

            def tr(e):
                for c in range(8):
                    ins = e.transpose(out=TB(tb)[:, c * 128:(c + 1) * 128], in_=ycat[:, c * 128:(c + 1) * 128], identity=ident[:])
                return ins
            A("pe", tr, r=["ycatA", "ycatB", "ident"], w=[f"G{tb}"])
            A("dve", lambda e: e.tensor_copy(out=yT[:].rearrange("p c t -> p (c t)"), in_=TB(tb)[:, :]), r=[f"G{tb}"], w=["yT"])

        def stage_Db(i):
            k = i % NXT
            for half in range(2):
                gb = nextG()

                def f(e, half=half, gb=gb):
                    for c in range(8):
                        ins = e.matmul(G[gb][:, :], lhsT=yT[:, c, :], rhs=wout[:, c, half * 512:(half + 1) * 512],
                                       start=(c == 0), stop=(c == 7))
                    return ins
                A("pe", f, r=["yT"] + WOUTKEYS, w=[f"G{gb}"])
                A("dve", lambda e, half=half, gb=gb: e.tensor_tensor(out=x1t[:, half * 512:(half + 1) * 512], in0=G[gb][:, :],
                                                                     in1=xt[k][:, half * 512:(half + 1) * 512], op=ALU.add),
                  r=[f"G{gb}", f"xt{k}"], w=[f"x1t{half}"])
            A("sp", lambda e: e.dma_start(out=x1_d[i * 128:(i + 1) * 128, :], in_=x1t[:]), r=["x1t0", "x1t1"], w=[f"x1d{i}"], dma="x1t")

        def stage_D(i):
            stage_Da(i)
            stage_Db(i)

        WINKEYS = [f"win{sl}_{cp}#{b_}" for sl in range(5) for cp in range(4) for b_ in range(2)]
        LOOPKEYS = WINKEYS + WOUTKEYS + ["Vones"] + [f"kT{j}" for j in range(NT)] + [f"V{j}" for j in range(NT)]
        hbs_by_half = [
            [(hb[:], ["hb"]), (hT[1][:].rearrange("p c t -> p (c t)"), ["hT1"])],
            [(hb[:], ["hb"]), (ycat[:], ["ycatA", "ycatB"])],
        ]

        def ffn_f1_pre(hf, tt):
            t = hf * 8 + tt
            xs = (t % 3) if hf == 0 else (t + 2) % NXT
            hbk = t % 2
            hbuf, hkeys = hbs_by_half[hf][hbk]
            A("sp", lambda e: e.dma_start(out=xt[xs][:], in_=x1_d[t * 128:(t + 1) * 128, :]),
              r=[f"x1d{t}"], w=[f"xt{xs}"], dma=f"xt{xs}")
            A("act", lambda e: e.activation(out=hbuf, in_=xt[xs][:], func=AF.Square, accum_out=st["ss2"][:, 0:1]),
              r=[f"xt{xs}"], w=["ss2", f"hbf{hbk}"] + hkeys)
            rstd_chain("ss2", "rs2", 1024.0, cols=1)
            A("dve", lambda e: e.scalar_tensor_tensor(out=hbuf, in0=xt[xs][:], scalar=st["rs2"][:, 0:1], in1=g2row,
                                                      op0=ALU.mult, op1=ALU.mult), r=[f"xt{xs}", "rs2", "grow2"],
              w=[f"hbf{hbk}"] + hkeys)

        def ffn_f1_post(hf, tt):
            t = hf * 8 + tt
            hbk = t % 2
            hbuf, hkeys = hbs_by_half[hf][hbk]
            tb = nextG()

            def tr(e):
                for c in range(8):
                    ins = e.transpose(out=TB(tb)[:, c * 128:(c + 1) * 128], in_=hbuf[:, c * 128:(c + 1) * 128], identity=ident[:])
                return ins
            A("pe", tr, r=[f"hbf{hbk}", "ident"], w=[f"G{tb}"])
            A("dve", lambda e: e.tensor_copy(out=h2T[:, :, tt * 128:(tt + 1) * 128], in_=v3(TB(tb)[:, :])),
              r=[f"G{tb}"], w=[f"h2T{tt}"] + WINKEYS)

        def ffn_half(hf):
            ffn_f1_pre(hf, 0)
            for tt in range(8):
                if tt + 1 < 8:
                    ffn_f1_pre(hf, tt + 1)
                ffn_f1_post(hf, tt)

        def ffn_prefetch(hf, f):
            ws = f % 2
            load_cast(wg_d[f, :, :], wgb[ws][:], 1024, [f"wgb{ws}", f"hT{ws}"], eng="act")
            load_cast(wu_d[f, :, :], wub[ws][:], 1024, [f"wub{ws}", f"qT{ws}", f"qTm{ws}"], eng="dve")
            if hf == 0:
                load_cast(wd_d[f, :, :], wdb[:, f, :], 1024, f"wdb{f}", eng="pool", rkeys=["ffnbar"])

        def run(fns):
            for fn in fns:
                fn()
                pump()

        stage_A0(0)
        stage_A0a(1)
        parts0 = A1_parts(0)
        win_pieces(2)
        run(parts0[0:2])
        win_pieces(3)
        run(parts0[2:4])
        win_pieces(0)
        win_pieces(1)
        run(parts0[4:8])
        win_pieces(4)
        run(parts0[8:10])
        A("sp", lambda e: e.dma_start(out=grow[:, 0:1024], in_=grow_d[:, 2048:3072]), w=["grow2"], dma="const2", total=True)
        tns_, keys_, dkey_ = slots["list"][cnt["st"] % len(slots["list"])]
        cnt["st"] += 1
        A("sp", lambda e: e.dma_start(out=tns_[:, :], in_=sgw_d[:, :]), w=keys_, dma=dkey_)
        A("dve", lambda e: e.tensor_tensor(out=wsg[:], in0=v3(tns_[:, :]), in1=causal.unsqueeze(1).to_broadcast([128, 8, 128]),
                                           op=ALU.mult), r=keys_ + ["causal"], w=["wsg"])
        slots["list"] = slots["list"][:NST]
        cnt["st"] = 0
        stage_A0b(1)
        load_x(2)
        load_x(3)
        stage_Bk(0)
        stage_Q1(0)
        stage_Q2(0)
        for i in range(NT):
            nun = 8 * ((i + 4) // 4)

            def P(fr, nun=nun):
                return int(fr * nun)
            parts = A1_parts(i + 1) if i + 1 < NT else [None] * 10
            groups = []
            if i == 0:
                for c in range(8):
                    groups.append([lambda c=c: load_cast(wout_d[:, c * 1024:(c + 1) * 1024], wout[:, c, :], 1024, f"wout{c}",
                                                         eng=("act", "dve")[c % 2], gain_cols=[8 + c])])
            groups.append([(lambda i=i: stage_A0a(i + 2)) if i + 2 < NT else None, parts[0]])
            for p_ in parts[1:7]:
                groups.append([p_])
            groups.append([parts[7],
                           (lambda i=i: stage_Da(i - 1)) if i >= 1 else None,
                           lambda i=i: stage_SGU(i),
                           parts[8]])
            groups.append([parts[9]])
            if i >= 1:
                def dfn(i=i):
                    stage_Db(i - 1)
                    if i + 3 < NT:
                        load_x(i + 3)
                groups.append([dfn])
            if i + 2 < NT:
                groups.append([lambda i=i: stage_A0b(i + 2)])
            if i == NT - 1:
                groups.append([lambda: ffn_prefetch(0, 0)])
                groups.append([lambda: ffn_f1_pre(0, 0)])
                for tt_ in range(8):
                    g_ = []
                    if tt_ + 1 < 8:
                        g_.append(lambda tt_=tt_: ffn_f1_pre(0, tt_ + 1))
                    g_.append(lambda tt_=tt_: ffn_f1_post(0, tt_))
                    groups.append(g_)
            groups = [[fn for fn in g_ if fn is not None] for g_ in groups]
            groups = [g_ for g_ in groups if g_]
            inserts = []
            for k_, g_ in enumerate(groups):
                for fn in g_:
                    inserts.append((P(0.04 + 0.71 * k_ / max(1, len(groups))), fn))
            if i + 1 < NT:
                p_ = max(P(0.76), inserts[-1][0])
                inserts.append((p_, lambda i=i: stage_Q1a(i + 1)))
                inserts.append((p_, lambda i=i: stage_Bk(i + 1)))
                inserts.append((max(P(0.86), p_ + 2), lambda i=i: stage_Q1b(i + 1)))
                inserts.append((nun - 1, lambda i=i: stage_Q2(i + 1)))
            stage_C(i, inserts)
            if debug and i == dbg_tile:
                s2 = i % 2
                for nm, src, keys in (("d_u", u_sbs[s2][:], [f"u{s2}"]), ("d_vln", vlns[s2][:], [f"vln{s2}"]),
                                      ("d_qaug", qaug[s2][:].rearrange("p h d -> p (h d)"), [f"qaug{s2}"]),
                                      ("d_kaug", kaug[s2][:].rearrange("p h d -> p (h d)"), [f"kaug{s2}", f"kaugoh{s2}"]),
                                      ("d_ycat", ycat[:], ["ycatA", "ycatB"]), ("d_ob", ob[:], ["ob0", "ob1"]),
                                      ("d_qT", qT[s2][:].rearrange("p h d -> p (h d)"), [f"qT{s2}", f"qTm{s2}"])):
                    A("sp", lambda e, nm=nm, src=src: e.dma_start(out=dbg[nm][:, :], in_=src), r=keys, dma="dbg", total=True)
        flush()

        def ffn_gu(hf, hook=None):
            def pe_act(f, ws, tg):
                def fg(e):
                    for c in range(8):
                        ins = e.matmul(SB[tg][:, :], lhsT=wgb[ws][:, c, :], rhs=h2T[:, c, tg * 512:(tg + 1) * 512],
                                       start=(c == 0), stop=(c == 7))
                    return ins
                A("pe", fg, r=[f"wgb{ws}"] + [f"h2T{4 * tg + q_}" for q_ in range(4)], w=[f"S{tg}"])

                def fu(e):
                    for c in range(8):
                        ins = e.matmul(O[tg][:, :], lhsT=wub[ws][:, c, :], rhs=h2T[:, c, tg * 512:(tg + 1) * 512],
                                       start=(c == 0), stop=(c == 7))
                    return ins
                A("pe", fu, r=[f"wub{ws}"] + [f"h2T{4 * tg + q_}" for q_ in range(4)], w=[f"O{tg}"] + (["O0misc"] if tg == 0 else []))
                A("act", lambda e: e.activation(out=sgs[tg][:], in_=SB[tg][:, :], func=AF.Silu), r=[f"S{tg}"], w=[f"sgs{tg}", ("qf", "kf")[tg]])

            def prod(f, tg):
                A("dve", lambda e: e.tensor_tensor(out=aT[:, f, tg * 512:(tg + 1) * 512], in0=O[tg][:, :], in1=sgs[tg][:],
                                                   op=ALU.mult), r=[f"O{tg}", f"sgs{tg}", "ffnbar"], w=[f"aT{f}_{tg}"])

            for f in range(NF):
                ws = f % 2
                if f == 0 and hook is not None:
                    pe_act(f, ws, 0)
                    pe_act(f, ws, 1)
                    hook()
                    ffn_prefetch(hf, 1)
                    prod(f, 0)
                    prod(f, 1)
                    continue
                if f + 1 < NF:
                    ffn_prefetch(hf, f + 1)
                for tg in range(2):
                    pe_act(f, ws, tg)
                    prod(f, tg)

        def ffn_down(hf):
            for tt in range(8):
                t = hf * 8 + tt
                xs = t % NXT
                A("sp", lambda e, t=t, xs=xs: e.dma_start(out=xt[xs][:], in_=x1_d[t * 128:(t + 1) * 128, :]),
                  r=[f"x1d{t}"], w=[f"xt{xs}"], dma=f"xt{xs}")
                if hf == 0:
                    ffn_f1_pre(1, tt)
                for dh in range(2):
                    gb = nextG()

                    def fd(e, gb=gb, dh=dh, tt=tt):
                        for f in range(NF):
                            ins = e.matmul(G[gb][:, :], lhsT=aT[:, f, tt * 128:(tt + 1) * 128], rhs=wdb[:, f, dh * 512:(dh + 1) * 512],
                                           start=(f == 0), stop=(f == NF - 1))
                        return ins
                    A("pe", fd, r=[f"aT{f}_{tt // 4}" for f in range(NF)] + [f"wdb{f}" for f in range(NF)], w=[f"G{gb}"])
                    A("dve", lambda e, gb=gb, dh=dh, xs=xs: e.tensor_tensor(out=ot[:, dh * 512:(dh + 1) * 512], in0=G[gb][:, :],
                                                                            in1=xt[xs][:, dh * 512:(dh + 1) * 512], op=ALU.add),
                      r=[f"G{gb}", f"xt{xs}", "ffnbar"], w=[f"x1t{dh}"])
                A("sp", lambda e, t=t: e.dma_start(out=out_d[t * 128:(t + 1) * 128, :], in_=ot[:]), r=["x1t0", "x1t1"], w=[f"outd{t}"], dma="ot")
                if hf == 0:
                    ffn_f1_post(1, tt)

        def end_of_loop():
            stage_D(NT - 1)
            flush()
            A("dve", lambda e: e.memset(st["ss2"][:, 1:2], 0.0), w=LOOPKEYS + ["ffnbar"], after_all=True)

        ffn_gu(0, hook=end_of_loop)
        ffn_prefetch(1, 0)
        ffn_down(0)
        ffn_gu(1)
        ffn_down(1)

        sems = {e: es.enter_context(nc.semaphore("s_" + e)) for e in Sched.ENGS}
        dsems = {k: es.enter_context(nc.semaphore("d_" + k)) for k in S.dkeys}
        S.assign(sems, dsems)
        finals = [S.dtotals["ot"]] + ([S.dtotals["dbg"]] if debug else [])
        with nc.Block() as block:
            @block.sync
            def _(e):
                S.run_engine("sp", e, final_waits=finals)

            @block.scalar
            def _(e):
                S.run_engine("act", e)

            @block.vector
            def _(e):
                S.run_engine("dve", e)

            @block.gpsimd
            def _(e):
                S.run_engine("pool", e)

            @block.tensor
            def _(e):
                S.run_engine("pe", e)
    return nc


_NC_CACHE = {}


def kernel(x, norm1_g, w_in, sgu_ln_g, sgu_ln_b, sgu_w, sgu_b, q_norm_g, k_norm_g,
           out_norm_a_g, out_norm_b_g, w_out, norm2_g, w_gate, w_up, w_down):
    f32 = np.float32
    x = np.asarray(x, f32)
    w_in_r = np.ascontiguousarray(np.asarray(w_in, f32)[0].reshape(8, 128, 2560).transpose(1, 0, 2)).reshape(128, 8 * 2560)
    w_out_r = np.ascontiguousarray(np.asarray(w_out, f32)[0].reshape(8, 128, 1024).transpose(1, 0, 2)).reshape(128, 8 * 1024)
    wg_r = np.ascontiguousarray(np.asarray(w_gate, f32)[0].reshape(8, 128, NF, 128).transpose(2, 1, 0, 3)).reshape(NF, 128, 1024)
    wu_r = np.ascontiguousarray(np.asarray(w_up, f32)[0].reshape(8, 128, NF, 128).transpose(2, 1, 0, 3)).reshape(NF, 128, 1024)
    wd_r = np.ascontiguousarray(np.asarray(w_down, f32)[0].reshape(NF, 128, 1024))
    sgu_wT = np.ascontiguousarray(np.asarray(sgu_w, f32)[0].transpose(2, 0, 1)).reshape(128, 1024)
    sgu_bT = np.ascontiguousarray(np.asarray(sgu_b, f32)[0].T)
    rowc = np.concatenate([np.asarray(sgu_ln_g, f32)[0], np.asarray(sgu_ln_b, f32)[0],
                           np.asarray(q_norm_g, f32)[0], np.asarray(k_norm_g, f32)[0]])
    rowc = np.ascontiguousarray(np.broadcast_to(rowc[None, :], (128, 1152)))
    grows = np.concatenate([np.asarray(norm1_g, f32)[0], np.asarray(out_norm_a_g, f32)[0], np.asarray(out_norm_b_g, f32)[0],
                            np.asarray(norm2_g, f32)[0]])
    grows = np.ascontiguousarray(np.broadcast_to(grows[None, :], (128, 3072)))
    gout = np.concatenate([np.asarray(out_norm_a_g, f32)[0], np.asarray(out_norm_b_g, f32)[0]])
    gcols = np.ascontiguousarray(np.concatenate([np.asarray(norm1_g, f32)[0].reshape(8, 128).T, gout.reshape(8, 128).T], axis=1))
    if "nc" not in _NC_CACHE:
        _NC_CACHE["nc"] = build_nc()
    nc = _NC_CACHE["nc"]
    shared = {"w_in_r": w_in_r, "w_out_r": w_out_r, "wg_r": wg_r, "wu_r": wu_r, "wd_r": wd_r, "sgu_wT": sgu_wT,
              "sgu_bT": sgu_bT, "rowc": rowc, "grows": grows, "gcols": gcols}
    in_maps = [dict(shared, x=np.ascontiguousarray(x[b])) for b in range(8)]
    res = run_bass_kernel_spmd(nc, in_maps, core_ids=list(range(8)))
    return np.stack([np.asarray(r["out"], f32) for r in res.results], axis=0)
```
